# Optimizing a Trainium2 kernel written in Bass

```python
import jax, jax.numpy as jnp
from jax import lax
import numpy as np

D_MODEL = 4096
BATCH = 4
SEQ = 2048
DEPTH = 1

MIX_WIDTH = D_MODEL
ATTN_HEAD_DIM = 128
ATTN_WIDTH = MIX_WIDTH // 2
ATTN_HEADS = ATTN_WIDTH // ATTN_HEAD_DIM
DILATED_BRANCHES = ((128, 1), (512, 4), (2048, 16))
ATTN_BLOCK = 128
ROPE_THETA = 10000.0
SSD_WIDTH = MIX_WIDTH - ATTN_WIDTH
SSD_HEAD_DIM = 64
SSD_HEADS = SSD_WIDTH // SSD_HEAD_DIM
SSD_GROUPS = 8
SSD_STATE = 128
SSD_CONV = 4
SSD_CHUNK = 256
SSD_CONV_DIM = SSD_WIDTH + 2 * SSD_GROUPS * SSD_STATE
IN_PROJ_WIDTH = 3 * ATTN_WIDTH + SSD_WIDTH + SSD_CONV_DIM + SSD_HEADS
IN_SPLITS = (ATTN_WIDTH, 2 * ATTN_WIDTH, 3 * ATTN_WIDTH,
             3 * ATTN_WIDTH + SSD_WIDTH, 3 * ATTN_WIDTH + SSD_WIDTH + SSD_CONV_DIM)
N_EXPERT_GROUPS = 4
EXPERTS_PER_GROUP = 8
N_EXPERTS = N_EXPERT_GROUPS * EXPERTS_PER_GROUP
TOP_K = 2
EXPERT_FF = D_MODEL // 4
MOE_BLOCK = 128
NORM_EPS = 1e-6
SSD_NORM_EPS = 1e-5

kernel_name = 'hymba_dilated_ssd_hmoe_block'


def rms_norm(x, w, eps):
    xf = x.astype(jnp.float32)
    y = xf * lax.rsqrt(jnp.mean(xf * xf, axis=-1, keepdims=True) + eps)
    return (y * w.astype(jnp.float32)).astype(x.dtype)


def rotary(t, positions):
    half = t.shape[-1] // 2
    inv_freq = jnp.power(jnp.float32(ROPE_THETA), -jnp.arange(half, dtype=jnp.float32) / half)
    ang = positions.astype(jnp.float32)[..., None] * inv_freq
    cos = jnp.cos(ang)[:, :, None, :]
    sin = jnp.sin(ang)[:, :, None, :]
    tf = t.astype(jnp.float32)
    t1, t2 = tf[..., :half], tf[..., half:]
    return jnp.concatenate([t1 * cos - t2 * sin, t2 * cos + t1 * sin], axis=-1).astype(t.dtype)


def dilated_branch(q, k, v, n_back, dil):
    b, s, h, dh = q.shape
    L = s // dil
    nb = -(-L // ATTN_BLOCK)
    lp = nb * ATTN_BLOCK

    def to_sub(t):
        t = t.reshape(b, L, dil, h, dh).transpose(0, 2, 3, 1, 4)
        t = jnp.pad(t, ((0, 0), (0, 0), (0, 0), (0, lp - L), (0, 0)))
        return t.reshape(b, dil, h, nb, ATTN_BLOCK, dh)

    def with_prev(t):
        prev = jnp.pad(t, ((0, 0), (0, 0), (0, 0), (1, 0), (0, 0), (0, 0)))[:, :, :, :-1]
        return jnp.concatenate([prev, t], axis=4)

    qs = to_sub(q)
    kk = with_prev(to_sub(k))
    vv = with_prev(to_sub(v))
    scale = jnp.float32(dh ** -0.5)
    sc = jnp.einsum('brhnqc,brhnkc->brhnqk', qs, kk).astype(jnp.float32) * scale
    qi = jnp.arange(ATTN_BLOCK)[:, None]
    kj = jnp.arange(2 * ATTN_BLOCK)[None, :]
    dist = ATTN_BLOCK + qi - kj
    kpos = (jnp.arange(nb)[:, None, None] - 1) * ATTN_BLOCK + kj[None]
    mask = (dist >= 0)[None] & (dist <= n_back)[None] & (kpos >= 0)
    sc = jnp.where(mask, sc, -jnp.inf)
    m = jnp.max(sc, axis=-1)
    p = jnp.exp(sc - m[..., None])
    l = jnp.sum(p, axis=-1)
    o = jnp.einsum('brhnqk,brhnkc->brhnqc', p, vv.astype(jnp.float32)) / l[..., None]
    o = o.reshape(b, dil, h, lp, dh)[:, :, :, :L].transpose(0, 3, 1, 2, 4).reshape(b, s, h, dh)
    back = lambda t: t.reshape(b, dil, h, lp)[..., :L].transpose(0, 3, 1, 2).reshape(b, s, h)
    return o, back(m), back(l)


def dilated_attention(q, k, v):
    outs, ms, ls = [], [], []
    for window, dil in DILATED_BRANCHES:
        o, m, l = dilated_branch(q, k, v, window // dil, dil)
        outs.append(o); ms.append(m); ls.append(l)
    m_all = jnp.stack(ms)
    wts = jnp.stack(ls) * jnp.exp(m_all - jnp.max(m_all, axis=0, keepdims=True))
    o = jnp.sum(wts[..., None] * jnp.stack(outs), axis=0) / jnp.sum(wts, axis=0)[..., None]
    return o.astype(q.dtype)


def causal_depthwise_conv(x, w, bias):
    c = x.shape[-1]
    y = lax.conv_general_dilated(x, w[:, None, :].astype(x.dtype), window_strides=(1,),
                                 padding=[(SSD_CONV - 1, 0)],
                                 dimension_numbers=('NWC', 'WIO', 'NWC'),
                                 feature_group_count=c)
    return y + bias.astype(x.dtype)


def ssd_chunked(x, dA, Bm, Cm):
    b, s, h, p = x.shape
    g, n = Bm.shape[2], Bm.shape[3]
    r = h // g
    nc = -(-s // SSD_CHUNK)
    sp = nc * SSD_CHUNK
    padseq = lambda t: jnp.pad(t, [(0, 0), (0, sp - s)] + [(0, 0)] * (t.ndim - 2))
    xc = padseq(x).reshape(b, nc, SSD_CHUNK, g, r, p)
    bc = padseq(Bm).reshape(b, nc, SSD_CHUNK, g, n)
    cc = padseq(Cm).reshape(b, nc, SSD_CHUNK, g, n)
    ac = padseq(dA).reshape(b, nc, SSD_CHUNK, g, r).transpose(0, 3, 4, 1, 2)
    cs = jnp.cumsum(ac, axis=-1)
    idx = jnp.arange(SSD_CHUNK)
    causal = idx[:, None] >= idx[None, :]
    seg = jnp.exp(jnp.where(causal, cs[..., :, None] - cs[..., None, :], -jnp.inf))
    cb = jnp.einsum('bclgn,bcsgn->bcgls', cc, bc)
    y_diag = jnp.einsum('bcgls,bgrcls,bcsgrp->bclgrp', cb, seg, xc)
    decay_states = jnp.exp(cs[..., -1:] - cs)
    states = jnp.einsum('bclgn,bgrcl,bclgrp->cbgrpn', bc, decay_states, xc)
    chunk_decay = jnp.exp(cs[..., -1]).transpose(3, 0, 1, 2)

    def step(h_prev, inp):
        dec, st = inp
        return dec[..., None, None] * h_prev + st, h_prev

    h0 = jnp.zeros((b, g, r, p, n), jnp.float32)
    _, prev = lax.scan(step, h0, (chunk_decay, states))
    y_off = jnp.einsum('bclgn,cbgrpn,bgrcl->bclgrp', cc, prev, jnp.exp(cs))
    return (y_diag + y_off).reshape(b, sp, h, p)[:, :s]


def ssd_mixer(z, xbc_raw, dt_raw, conv_w, conv_b, dt_bias, a_log, d_skip, norm_w):
    b, s, _ = z.shape
    xbc = jax.nn.silu(causal_depthwise_conv(xbc_raw, conv_w, conv_b))
    xs, bm, cm = jnp.split(xbc, [SSD_WIDTH, SSD_WIDTH + SSD_GROUPS * SSD_STATE], axis=-1)
    xs = xs.reshape(b, s, SSD_HEADS, SSD_HEAD_DIM).astype(jnp.float32)
    bm = bm.reshape(b, s, SSD_GROUPS, SSD_STATE).astype(jnp.float32)
    cm = cm.reshape(b, s, SSD_GROUPS, SSD_STATE).astype(jnp.float32)
    dt = jax.nn.softplus(dt_raw.astype(jnp.float32) + dt_bias.astype(jnp.float32))
    a = -jnp.exp(a_log.astype(jnp.float32))
    y = ssd_chunked(xs * dt[..., None], dt * a, bm, cm)
    y = y + d_skip.astype(jnp.float32)[:, None] * xs
    y = y.reshape(b, s, SSD_WIDTH) * jax.nn.silu(z.astype(jnp.float32))
    yg = y.reshape(b, s, SSD_GROUPS, SSD_WIDTH // SSD_GROUPS)
    yg = yg * lax.rsqrt(jnp.mean(yg * yg, axis=-1, keepdims=True) + SSD_NORM_EPS)
    y = yg.reshape(b, s, SSD_WIDTH) * norm_w.astype(jnp.float32)
    return y.astype(z.dtype)


def hierarchical_moe(h, router_group_w, router_group_b, router_expert_w, router_expert_b,
                     w_gate, w_up, w_down):
    t, d = h.shape
    hf = h.astype(jnp.float32)
    g_probs = jax.nn.softmax(hf @ router_group_w.astype(jnp.float32) + router_group_b.astype(jnp.float32), axis=-1)
    g_p, g_idx = lax.top_k(g_probs, 1)
    e_logits = (hf @ router_expert_w.astype(jnp.float32) + router_expert_b.astype(jnp.float32))
    e_logits = e_logits.reshape(t, N_EXPERT_GROUPS, EXPERTS_PER_GROUP)
    e_sel = jnp.take_along_axis(e_logits, g_idx[:, :, None], axis=1)[:, 0]
    e_p, e_local = lax.top_k(jax.nn.softmax(e_sel, axis=-1), TOP_K)
    e_p = e_p / jnp.sum(e_p, axis=-1, keepdims=True)
    gates = g_p * e_p
    expert_ids = g_idx * EXPERTS_PER_GROUP + e_local

    n_assign = t * TOP_K
    flat_e = expert_ids.reshape(n_assign)
    flat_tok = jnp.repeat(jnp.arange(t, dtype=jnp.int32), TOP_K)
    flat_gate = gates.reshape(n_assign)
    order = jnp.argsort(flat_e, stable=True)
    e_sorted, tok_sorted, gate_sorted = flat_e[order], flat_tok[order], flat_gate[order]
    counts = jnp.bincount(flat_e, length=N_EXPERTS)
    starts = jnp.cumsum(counts) - counts
    padded = ((counts + MOE_BLOCK - 1) // MOE_BLOCK) * MOE_BLOCK
    pstarts = jnp.cumsum(padded) - padded
    dest = pstarts[e_sorted] + (jnp.arange(n_assign) - starts[e_sorted])
    n_blocks = n_assign // MOE_BLOCK + N_EXPERTS
    rows = n_blocks * MOE_BLOCK
    row_tok = jnp.full((rows,), t, jnp.int32).at[dest].set(tok_sorted)
    row_gate = jnp.zeros((rows,), jnp.float32).at[dest].set(gate_sorted)
    block_start = jnp.arange(n_blocks) * MOE_BLOCK
    block_e = jnp.minimum(jnp.sum(block_start[:, None] >= (pstarts + padded)[None, :], axis=1), N_EXPERTS - 1)
    h_pad = jnp.concatenate([h, jnp.zeros((1, d), h.dtype)], axis=0)
    xb = h_pad[row_tok].reshape(n_blocks, MOE_BLOCK, d)

    def expert_block(args):
        xblk, e = args
        return (jax.nn.silu(xblk @ w_gate[e]) * (xblk @ w_up[e])) @ w_down[e]

    yb = lax.map(expert_block, (xb, block_e))
    y_rows = yb.reshape(rows, d) * row_gate[:, None].astype(yb.dtype)
    return jax.ops.segment_sum(y_rows, row_tok, num_segments=t + 1)[:t]


def hybrid_layer(x, positions, norm_attn_w, w_in, q_norm_w, k_norm_w, conv_w, conv_b,
                 dt_bias, a_log, d_skip, ssd_norm_w, w_out, norm_ffn_w,
                 router_group_w, router_group_b, router_expert_w, router_expert_b,
                 w_gate, w_up, w_down):
    b, s, d = x.shape
    h = rms_norm(x, norm_attn_w, NORM_EPS)
    proj = jnp.einsum('bsd,de->bse', h, w_in)
    q, k, v, z, xbc, dt_raw = jnp.split(proj, IN_SPLITS, axis=-1)
    q = rotary(rms_norm(q.reshape(b, s, ATTN_HEADS, ATTN_HEAD_DIM), q_norm_w, NORM_EPS), positions)
    k = rotary(rms_norm(k.reshape(b, s, ATTN_HEADS, ATTN_HEAD_DIM), k_norm_w, NORM_EPS), positions)
    v = v.reshape(b, s, ATTN_HEADS, ATTN_HEAD_DIM)
    attn = dilated_attention(q, k, v).reshape(b, s, ATTN_WIDTH)
    ssd = ssd_mixer(z, xbc, dt_raw, conv_w, conv_b, dt_bias, a_log, d_skip, ssd_norm_w)
    mix = jnp.concatenate([attn, ssd], axis=-1)
    x = x + jnp.einsum('bse,ed->bsd', mix, w_out)
    h2 = rms_norm(x, norm_ffn_w, NORM_EPS).reshape(b * s, d)
    y = hierarchical_moe(h2, router_group_w, router_group_b, router_expert_w, router_expert_b,
                         w_gate, w_up, w_down)
    return x + y.reshape(b, s, d)


def setup_inputs(seed: int = 0) -> dict:
    key = jax.random.key(seed)
    ks = jax.random.split(key, 24)
    f32 = jnp.float32
    nrm = lambda k, shape, sc: jax.random.normal(k, shape, f32) * sc
    x = jax.random.normal(ks[0], (BATCH, SEQ, D_MODEL), f32)
    positions = jnp.tile(jnp.arange(SEQ, dtype=jnp.int32)[None, :], (BATCH, 1))
    dt0 = jnp.exp(jax.random.uniform(ks[8], (DEPTH, SSD_HEADS), f32, np.log(1e-3), np.log(1e-1)))
    return {
        'x': x,
        'positions': positions,
        'norm_attn_w': 1.0 + nrm(ks[1], (DEPTH, D_MODEL), 0.02),
        'w_in': nrm(ks[2], (DEPTH, D_MODEL, IN_PROJ_WIDTH), D_MODEL ** -0.5),
        'q_norm_w': 1.0 + nrm(ks[3], (DEPTH, ATTN_HEAD_DIM), 0.02),
        'k_norm_w': 1.0 + nrm(ks[4], (DEPTH, ATTN_HEAD_DIM), 0.02),
        'conv_w': nrm(ks[5], (DEPTH, SSD_CONV, SSD_CONV_DIM), SSD_CONV ** -0.5),
        'conv_b': nrm(ks[6], (DEPTH, SSD_CONV_DIM), 0.01),
        'dt_bias': dt0 + jnp.log(-jnp.expm1(-dt0)),
        'a_log': jnp.log(jax.random.uniform(ks[9], (DEPTH, SSD_HEADS), f32, 1.0, 16.0)),
        'd_skip': 1.0 + nrm(ks[10], (DEPTH, SSD_HEADS), 0.1),
        'ssd_norm_w': 1.0 + nrm(ks[11], (DEPTH, SSD_WIDTH), 0.02),
        'w_out': nrm(ks[12], (DEPTH, MIX_WIDTH, D_MODEL), MIX_WIDTH ** -0.5),
        'norm_ffn_w': 1.0 + nrm(ks[13], (DEPTH, D_MODEL), 0.02),
        'router_group_w': nrm(ks[14], (DEPTH, D_MODEL, N_EXPERT_GROUPS), D_MODEL ** -0.5),
        'router_group_b': nrm(ks[15], (DEPTH, N_EXPERT_GROUPS), 0.01),
        'router_expert_w': nrm(ks[16], (DEPTH, D_MODEL, N_EXPERTS), D_MODEL ** -0.5),
        'router_expert_b': nrm(ks[17], (DEPTH, N_EXPERTS), 0.01),
        'w_gate': nrm(ks[18], (DEPTH, N_EXPERTS, D_MODEL, EXPERT_FF), D_MODEL ** -0.5),
        'w_up': nrm(ks[19], (DEPTH, N_EXPERTS, D_MODEL, EXPERT_FF), D_MODEL ** -0.5),
        'w_down': nrm(ks[20], (DEPTH, N_EXPERTS, EXPERT_FF, D_MODEL), EXPERT_FF ** -0.5),
    }


def reference(x, positions, norm_attn_w, w_in, q_norm_w, k_norm_w, conv_w, conv_b,
              dt_bias, a_log, d_skip, ssd_norm_w, w_out, norm_ffn_w,
              router_group_w, router_group_b, router_expert_w, router_expert_b,
              w_gate, w_up, w_down):
    for layer in range(DEPTH):
        x = hybrid_layer(x, positions, norm_attn_w[layer], w_in[layer], q_norm_w[layer],
                         k_norm_w[layer], conv_w[layer], conv_b[layer], dt_bias[layer],
                         a_log[layer], d_skip[layer], ssd_norm_w[layer], w_out[layer],
                         norm_ffn_w[layer], router_group_w[layer], router_group_b[layer],
                         router_expert_w[layer], router_expert_b[layer],
                         w_gate[layer], w_up[layer], w_down[layer])
    return x
```

```python
import numpy as np
from contextlib import ExitStack
import concourse.bass as bass
import concourse.mybir as mybir
from concourse.bass_utils import run_bass_kernel_spmd

F32 = mybir.dt.float32
BF16 = mybir.dt.bfloat16
I32 = mybir.dt.int32
AF = mybir.ActivationFunctionType
ALU = mybir.AluOpType
AX = mybir.AxisListType

D = 4096
NTOK = 1024
NT = NTOK // 128
KC = D // 128
NH = 16
NE = 32
CE = 128
FF = 1024
TWO_PI = 6.283179


class Buf:
    __slots__ = ("name", "lw", "rd", "psum")

    def __init__(self, name, psum=False):
        self.name = name
        self.lw = {}
        self.rd = {}
        self.psum = psum


class Sched:
    ENG = ("pe", "act", "dve", "pool", "sp")

    def __init__(self, nc, es):
        self.nc = nc
        self.es = es
        self.e = {"pe": nc.tensor, "act": nc.scalar, "dve": nc.vector, "pool": nc.gpsimd, "sp": nc.sync}
        self.csem = {k: es.enter_context(nc.semaphore("c_" + k)) for k in ("pe", "act", "dve", "pool")}
        self.ccnt = {k: 0 for k in self.csem}
        self.dsems = []
        self.known = {k: {} for k in self.ENG}
        self.nwaits = 0
        self.bg_chans = set()

    def chan(self):
        s = self.es.enter_context(self.nc.semaphore("d%d" % len(self.dsems)))
        self.dsems.append([s, 0])
        return len(self.dsems) - 1

    def _wait(self, E, kk, val):
        kind, key = kk
        if kind == "c":
            if key == "pe" and E == "pe":
                return
            if val > self.ccnt[key]:
                raise RuntimeError("wait on an event that is not issued yet: %s %d" % (key, val))
            sem = self.csem[key]
        else:
            sem, val = self.dsems[key]
        if self.known[E].get(kk, 0) >= val:
            return
        self.e[E].wait_ge(sem, val)
        self.nwaits += 1
        self.known[E][kk] = val

    def _deps(self, E, reads, writes):
        need = {}
        for b in reads:
            for kk, v in b.lw.items():
                need[kk] = max(need.get(kk, 0), v)
        for b in writes:
            for kk, v in b.lw.items():
                need[kk] = max(need.get(kk, 0), v)
            for kk, v in b.rd.items():
                need[kk] = max(need.get(kk, 0), v)
        for kk, v in need.items():
            self._wait(E, kk, v)

    def _record(self, kk, val, reads, writes):
        for b in reads:
            b.rd[kk] = max(b.rd.get(kk, 0), val)
        for b in writes:
            b.lw = {kk: val}
            b.rd = {}

    def op(self, E, fn, reads=(), writes=(), inc=True):
        if E != "pe":
            ex = [b for b in reads if b.psum]
            if ex:
                writes = list(writes) + ex
        self._deps(E, reads, writes)
        ins = fn()
        if inc:
            self.ccnt[E] += 1
            ins.then_inc(self.csem[E], 1)
            val = self.ccnt[E]
        else:
            assert E == "pe"
            val = self.ccnt[E] + 1
        self._record(("c", E), val, reads, writes)
        return ins

    def dma(self, Q, fn, ch, reads=(), writes=()):
        self._deps(Q, reads, writes)
        ins = fn()
        self.dsems[ch][1] += 16
        ins.then_inc(self.dsems[ch][0], 16)
        self._record(("d", ch), self.dsems[ch][1], reads, writes)
        return ins

    def barrier(self, final=False):
        for E in self.ENG:
            for k in self.csem:
                self._wait(E, ("c", k), self.ccnt[k])
            for i in range(len(self.dsems)):
                if i in self.bg_chans and not final:
                    continue
                self._wait(E, ("d", i), 0)


_UID = [0]


class Rot:
    def __init__(self, S, st, name, shape, dtype, n, dma=False):
        _UID[0] += 1
        self.t = [st.enter_context(S.nc.sbuf_tensor("%s_%d_%d" % (name, _UID[0], i), shape, dtype)) for i in range(n)]
        self.b = [Buf("%s%d" % (name, i)) for i in range(n)]
        self.ch = [S.chan() for _ in range(n)] if dma else None
        self.i = -1
        self.n = n

    def next(self):
        self.i = (self.i + 1) % self.n
        if self.ch is None:
            return self.t[self.i], self.b[self.i]
        return self.t[self.i], self.b[self.i], self.ch[self.i]


def build_program(dbg=None):
    dbg = dbg or {}
    stop = dbg.get("stop")
    exported = dbg.get("export", ())
    nc = bass.Bass("TRN2", target_bir_lowering=False)
    es = ExitStack()
    S = Sched(nc, es)

    def din(name, shape, dt=F32):
        if name in dbg.get("dummy", ()):
            shape = [1, 1]
        return nc.dram_tensor(name, list(shape), dt, kind="ExternalInput").ap()

    inject = dbg.get("inject", ())

    def dscr(name, shape, dt):
        kind = "ExternalOutput" if name in exported else ("ExternalInput" if name in inject else "Internal")
        return nc.dram_tensor(name, list(shape), dt, kind=kind).ap()

    x_own = din("x_own", [NTOK, D])
    x_pre = din("x_pre", [NTOK, D])
    pos_own = din("pos_own", [NTOK], I32)
    pos_pre = din("pos_pre", [NTOK], I32)
    vflag_d = din("vflag", [128, NT])
    w_in = din("w_in", [D, 12320])
    w_out = din("w_out", [D, D])
    need_moe = stop in (None, "G")
    if need_moe:
        nexp = len(dbg.get("experts", range(NE)))
        w_gate = din("w_gate", [nexp, D, FF])
        w_up = din("w_up", [nexp, D, FF])
        w_down = din("w_down", [nexp, FF, D])
    norm_attn_w = din("norm_attn_w", [D])
    norm_ffn_w = din("norm_ffn_w", [D])
    nfw_col = din("nfw_col", [128, KC])
    qk_w = din("qk_w", [128, 2])
    conv_wc = din("conv_wc", [128, 32, 4])
    conv_bc = din("conv_bc", [128, 32])
    dt_bias = din("dt_bias", [32])
    a_log = din("a_log", [32])
    d_rep = din("d_rep", [2048])
    ssd_nw = din("ssd_nw", [2048])
    w_r = din("w_r", [128, KC, 36])
    b_r = din("b_r", [36])
    c_ident = din("c_ident", [128, 128])
    c_pswap = din("c_pswap", [128, 128])
    c_tri = din("c_tri", [128, 128])
    c_stri = din("c_stri", [128, 128])
    c_negm = din("c_negm", [128, 128])
    c_amask = din("c_amask", [128, 16 * 128])
    c_rope = din("c_rope", [128, 2])
    c_ebase = din("c_ebase", [128, NE])
    out_d = nc.dram_tensor("out", [NTOK, D], F32, kind="ExternalOutput").ap()

    qT_d = dscr("s_qT", [NH, 128, NTOK], BF16)
    kT_d = dscr("s_kT", [NH, 128, 2 * NTOK], BF16)
    v_d = dscr("s_v", [2 * NTOK, 2048], BF16)
    zs_d = dscr("s_zs", [NTOK, 2048], BF16)
    xbcT_d = dscr("s_xbcT", [4096, 2 * NTOK], F32)
    dtr_d = dscr("s_dtr", [2 * NTOK, 32], F32)
    mix_d = dscr("s_mix", [NTOK, D], BF16)
    x2_d = dscr("s_x2", [NTOK, D], F32)
    xs_d = dscr("s_xs", [NE * CE, D], BF16)
    ys_d = dscr("s_ys", [NE * CE, D], F32)
    dbg_d = dscr("s_dbg", [NTOK, 64], F32)

    def sb(st, name, shape, dt):
        _UID[0] += 1
        return st.enter_context(nc.sbuf_tensor("%s_%d" % (name, _UID[0]), list(shape), dt))

    ident_f = sb(es, "ident_f", [128, 128], F32)
    ident_b = sb(es, "ident_b", [128, 128], BF16)
    ones_b = sb(es, "ones_b", [128, 128], BF16)
    ones_f = sb(es, "ones_f", [128, 128], F32)
    pswap_b = sb(es, "pswap_b", [128, 128], BF16)
    tri_f = sb(es, "tri_f", [128, 128], F32)
    stri_b = sb(es, "stri_b", [128, 128], BF16)
    negm_f = sb(es, "negm_f", [128, 128], F32)
    amask = sb(es, "amask", [128, 16 * 128], BF16)
    rope_c = sb(es, "rope_c", [128, 2], F32)
    qkw = sb(es, "qkw", [128, 2], F32)
    vflag = sb(es, "vflag_sb", [128, NT], F32)
    eps6 = sb(es, "eps6", [128, 1], F32)
    eps5 = sb(es, "eps5", [128, 1], F32)
    zero_b = sb(es, "zero_b", [128, D], BF16)
    CB = Buf("consts")
    cch = S.chan()
    for t, src in ((ident_f, c_ident), (tri_f, c_tri), (negm_f, c_negm), (rope_c, c_rope), (qkw, qk_w),
                   (vflag, vflag_d)):
        S.dma("sp", lambda t=t, src=src: nc.sync.dma_start(out=t[:], in_=src), cch, writes=[CB])
    for t, src in ((ident_b, c_ident), (pswap_b, c_pswap), (stri_b, c_stri), (amask, c_amask)):
        S.dma("pool", lambda t=t, src=src: nc.gpsimd.dma_start(out=t[:], in_=src), cch, writes=[CB])
    S.op("dve", lambda: nc.vector.memset(ones_b[:], 1.0), writes=[CB])
    S.op("dve", lambda: nc.vector.memset(ones_f[:], 1.0), writes=[CB])
    S.op("dve", lambda: nc.vector.memset(eps6[:], 1e-6), writes=[CB])
    S.op("dve", lambda: nc.vector.memset(eps5[:], 1e-5), writes=[CB])
    S.op("dve", lambda: nc.vector.memset(zero_b[:], 0.0), writes=[CB])
    S.op("dve", lambda: nc.vector.tensor_scalar(out=qkw[:, 0:1], in0=qkw[:, 0:1], scalar1=float(128 ** -0.5),
                                                scalar2=None, op0=ALU.mult), reads=[CB], writes=[CB])
    zch = S.chan()
    for e in range(NE):
        S.dma("sp", lambda e=e: nc.sync.dma_start(out=xs_d[e * CE:(e + 1) * CE, :], in_=zero_b[:]), zch, reads=[CB])

    ps = [es.enter_context(nc.psum_tensor("ps%d" % i, [128, 512], F32)) for i in range(8)]
    pb = [Buf("ps%d" % i, psum=True) for i in range(8)]
    S.barrier()

    def proj_pass(x_d, pos_d, own):
        tok0 = NTOK if own else 0
        with ExitStack() as st:
            hT = sb(st, "hT", [128, KC, NTOK], BF16)
            hTb = [Buf("hT%d" % i) for i in range(NT)]
            cosT = sb(st, "cosT", [128, NTOK], F32)
            sinT = sb(st, "sinT", [128, NTOK], F32)
            TB = Buf("rope_tab")
            with ExitStack() as sa:
                wbc = sb(sa, "wbc", [128, D], F32)
                WB = Buf("wbc")
                S.dma("sp", lambda: nc.sync.dma_start(out=wbc[:], in_=norm_attn_w.partition_broadcast(128)),
                      cch, writes=[WB])
                posi = sb(sa, "posi", [128, NTOK], I32)
                yv = sb(sa, "yv", [128, NTOK], F32)
                yi = sb(sa, "yi", [128, NTOK], I32)
                yf = sb(sa, "yf", [128, NTOK], F32)
                S.dma("sp", lambda: nc.sync.dma_start(out=posi[:], in_=pos_d.partition_broadcast(128)), cch,
                      writes=[TB])
                S.op("dve", lambda: nc.vector.tensor_copy(out=yf[:], in_=posi[:]), reads=[TB], writes=[TB])
                S.op("dve", lambda: nc.vector.tensor_scalar(out=yv[:], in0=yf[:], scalar1=rope_c[:, 0:1],
                                                            scalar2=None, op0=ALU.mult), reads=[TB, CB], writes=[TB])
                for tab, shift in ((sinT, 0.0), (cosT, 0.25)):
                    if shift:
                        S.op("dve", lambda: nc.vector.tensor_scalar(out=yv[:], in0=yv[:], scalar1=shift,
                                                                    scalar2=None, op0=ALU.add), reads=[TB], writes=[TB])
                    S.op("dve", lambda: nc.vector.tensor_copy(out=yi[:], in_=yv[:]), reads=[TB], writes=[TB])
                    S.op("dve", lambda: nc.vector.tensor_copy(out=yf[:], in_=yi[:]), reads=[TB], writes=[TB])
                    S.op("dve", lambda: nc.vector.tensor_tensor(out=yf[:], in0=yv[:], in1=yf[:], op=ALU.subtract),
                         reads=[TB], writes=[TB])
                    if shift:
                        S.op("act", lambda tab=tab: nc.scalar.activation(out=tab[:], in_=yf[:], func=AF.Sin,
                                                                         scale=TWO_PI), reads=[TB], writes=[TB])
                    else:
                        S.op("act", lambda tab=tab: nc.scalar.activation(out=tab[:], in_=yf[:], func=AF.Sin,
                                                                         scale=rope_c[:, 1:2]), reads=[TB, CB], writes=[TB])
                xt = Rot(S, sa, "xt", [128, D], F32, 2, dma=True)
                hb = Rot(S, sa, "hb", [128, D], BF16, 2)
                junk = sb(sa, "junk", [128, D], BF16)
                JB = Buf("junk")
                ssr = Rot(S, sa, "ss", [128, 2], F32, 2)
                for tt in range(NT):
                    x_t, x_b, x_ch = xt.next()
                    S.dma("sp", lambda x_t=x_t, tt=tt: nc.sync.dma_start(out=x_t[:], in_=x_d[tt * 128:(tt + 1) * 128, :]),
                          x_ch, writes=[x_b])
                    s_t, s_b = ssr.next()
                    S.op("act", lambda x_t=x_t, s_t=s_t: nc.scalar.activation(out=junk[:], in_=x_t[:], func=AF.Square,
                                                                              accum_out=s_t[:, 0:1]),
                         reads=[x_b], writes=[JB, s_b])
                    S.op("act", lambda s_t=s_t: nc.scalar.activation(out=s_t[:, 1:2], in_=s_t[:, 0:1], func=AF.Sqrt,
                                                                     scale=1.0 / D, bias=eps6[:, 0:1]),
                         reads=[s_b, CB], writes=[s_b])
                    S.op("dve", lambda s_t=s_t: nc.vector.reciprocal(out=s_t[:, 1:2], in_=s_t[:, 1:2]), reads=[s_b], writes=[s_b])
                    h_t, h_b = hb.next()
                    S.op("dve", lambda h_t=h_t, x_t=x_t, s_t=s_t: nc.vector.scalar_tensor_tensor(
                        out=h_t[:], in0=x_t[:], scalar=s_t[:, 1:2], in1=wbc[:], op0=ALU.mult, op1=ALU.mult),
                        reads=[x_b, s_b, WB], writes=[h_b])
                    for q4 in range(4):
                        bank = q4 % 4
                        pbf = ps[bank][:].bitcast(BF16)
                        for j in range(8):
                            kc = q4 * 8 + j
                            S.op("pe", lambda pbf=pbf, j=j, kc=kc, h_t=h_t: nc.tensor.transpose(
                                out=pbf[:, j * 128:(j + 1) * 128], in_=h_t[:, kc * 128:(kc + 1) * 128], identity=ident_b[:]),
                                reads=[h_b, CB], writes=[pb[bank]], inc=(j == 7))
                        src = pbf.rearrange("p (j t) -> p j t", j=8)
                        dst = hT[:, q4 * 8:(q4 + 1) * 8, tt * 128:(tt + 1) * 128]
                        if q4 % 2 == 0:
                            S.op("act", lambda dst=dst, src=src: nc.scalar.copy(out=dst, in_=src), reads=[pb[bank]], writes=[hTb[tt]])
                        else:
                            S.op("dve", lambda dst=dst, src=src: nc.vector.tensor_copy(out=dst, in_=src), reads=[pb[bank]], writes=[hTb[tt]])
                S.barrier()
            if stop == "A":
                return
            wv = w_in.rearrange("(kc p) n -> p kc n", p=128)
            wt = Rot(S, st, "wt", [128, KC, 512], BF16, 2, dma=True)
            ev32 = Rot(S, st, "ev32", [128, 512], F32, 3, dma=True)
            evb = Rot(S, st, "evb", [128, 512], BF16, 3, dma=True)
            sqr = Rot(S, st, "sq", [128, 512], BF16, 2)
            rsr = Rot(S, st, "rs", [128, 512], F32, 2)
            qnr = Rot(S, st, "qn", [128, 512], BF16, 2)
            t1r = Rot(S, st, "t1", [128, 512], F32, 2)
            t2r = Rot(S, st, "t2", [128, 512], F32, 2)
            items = []
            if own:
                items += [("q", c0) for c0 in range(0, 2048, 512)]
            items += [("k", c0) for c0 in range(2048, 4096, 512)]
            items += [("v", c0) for c0 in range(4096, 6144, 512)]
            if own:
                items += [("z", c0) for c0 in range(6144, 8192, 512)]
            items += [("f", c0) for c0 in range(8192, 12288, 512)]
            items += [("dt", 12288)]
            if dbg.get("items"):
                items = [it for it in items if it[0] in dbg["items"]]
            mainbank = [0]
            auxbank = [0, 0]
            pending = []

            def step_pending(flush=False):
                while True:
                    for g in list(pending):
                        try:
                            next(g)
                        except StopIteration:
                            pending.remove(g)
                    if not flush or not pending:
                        break

            def qk_post(kind, head, tb, bank):
                wcol = qkw[:, 0:1] if kind == "q" else qkw[:, 1:2]
                sq_t, sq_b = sqr.next()
                S.op("act", lambda: nc.scalar.activation(out=sq_t[:], in_=ps[bank][:], func=AF.Square),
                     reads=[pb[bank]], writes=[sq_b])
                yield
                b2 = 4 + auxbank[0] % 2
                auxbank[0] += 1
                S.op("pe", lambda: nc.tensor.matmul(ps[b2][:], lhsT=ones_b[:], rhs=sq_t[:], start=True, stop=True),
                     reads=[sq_b, CB], writes=[pb[b2]])
                rs_t, rs_b = rsr.next()
                S.op("act", lambda: nc.scalar.activation(out=rs_t[:], in_=ps[b2][:], func=AF.Sqrt, scale=1.0 / 128,
                                                         bias=eps6[:, 0:1]), reads=[pb[b2], CB], writes=[rs_b])
                S.op("dve", lambda: nc.vector.reciprocal(out=rs_t[:], in_=rs_t[:]), reads=[rs_b], writes=[rs_b])
                qn_t, qn_b = qnr.next()
                S.op("dve", lambda: nc.vector.scalar_tensor_tensor(out=qn_t[:], in0=ps[bank][:], scalar=wcol, in1=rs_t[:],
                                                                   op0=ALU.mult, op1=ALU.mult),
                     reads=[pb[bank], rs_b, CB], writes=[qn_b])
                yield
                b3 = 6 + auxbank[1] % 2
                auxbank[1] += 1
                S.op("pe", lambda: nc.tensor.matmul(ps[b3][:], lhsT=pswap_b[:], rhs=qn_t[:], start=True, stop=True),
                     reads=[qn_b, CB], writes=[pb[b3]])
                t1_t, t1_b = t1r.next()
                t2_t, t2_b = t2r.next()
                cs = slice(tb * 512, (tb + 1) * 512)
                S.op("dve", lambda: nc.vector.tensor_tensor(out=t1_t[:], in0=qn_t[:], in1=cosT[:, cs], op=ALU.mult),
                     reads=[qn_b, TB], writes=[t1_b])
                S.op("dve", lambda: nc.vector.tensor_tensor(out=t2_t[:], in0=ps[b3][:], in1=sinT[:, cs], op=ALU.mult),
                     reads=[pb[b3], TB], writes=[t2_b])
                o_t, o_b, o_ch = evb.next()
                S.op("dve", lambda: nc.vector.tensor_tensor(out=o_t[:], in0=t1_t[:], in1=t2_t[:], op=ALU.add),
                     reads=[t1_b, t2_b], writes=[o_b])
                if kind == "q":
                    dst = qT_d[head, :, tb * 512:(tb + 1) * 512]
                else:
                    dst = kT_d[head, :, tok0 + tb * 512: tok0 + (tb + 1) * 512]
                S.dma("sp", lambda: nc.sync.dma_start(out=dst, in_=o_t[:]), o_ch, reads=[o_b])

            for kind, c0 in items:
                ncol = 32 if kind == "dt" else 512
                w_t, w_b, w_ch = wt.next()
                S.dma("pool", lambda w_t=w_t, c0=c0, ncol=ncol: nc.gpsimd.dma_start(
                    out=w_t[:, :, 0:ncol], in_=wv[:, :, c0:c0 + ncol]), w_ch, writes=[w_b])
                if NCV_CHUNKS and kind != "dt":
                    bg_convert(1)
                if kind in ("q", "k", "f"):
                    for sub in range(4):
                        for tb in range(2):
                            bank = mainbank[0] % 4
                            mainbank[0] += 1
                            for kc in range(KC):
                                S.op("pe", lambda bank=bank, kc=kc, sub=sub, tb=tb, w_t=w_t: nc.tensor.matmul(
                                    ps[bank][:], lhsT=w_t[:, kc, sub * 128:(sub + 1) * 128],
                                    rhs=hT[:, kc, tb * 512:(tb + 1) * 512], start=(kc == 0), stop=(kc == KC - 1)),
                                    reads=[w_b] + hTb[tb * 4:(tb + 1) * 4], writes=[pb[bank]], inc=(kc == KC - 1))
                            if kind == "f":
                                e_t, e_b, e_ch = ev32.next()
                                if (sub + tb) % 2 == 0:
                                    S.op("act", lambda e_t=e_t, bank=bank: nc.scalar.copy(out=e_t[:], in_=ps[bank][:]),
                                         reads=[pb[bank]], writes=[e_b])
                                else:
                                    S.op("dve", lambda e_t=e_t, bank=bank: nc.vector.tensor_copy(out=e_t[:], in_=ps[bank][:]),
                                         reads=[pb[bank]], writes=[e_b])
                                ch0 = c0 - 8192 + sub * 128
                                S.dma("sp", lambda e_t=e_t, ch0=ch0, tb=tb: nc.sync.dma_start(
                                    out=xbcT_d[ch0:ch0 + 128, tok0 + tb * 512: tok0 + (tb + 1) * 512], in_=e_t[:]),
                                    e_ch, reads=[e_b])
                            else:
                                head = ((c0 - (0 if kind == "q" else 2048)) // 128) + sub
                                pending.append(qk_post(kind, head, tb, bank))
                            step_pending()
                else:
                    for tt in range(NT):
                        bank = mainbank[0] % 4
                        mainbank[0] += 1
                        for kc in range(KC):
                            S.op("pe", lambda bank=bank, kc=kc, tt=tt, w_t=w_t, ncol=ncol: nc.tensor.matmul(
                                ps[bank][:, 0:ncol], lhsT=hT[:, kc, tt * 128:(tt + 1) * 128], rhs=w_t[:, kc, 0:ncol],
                                start=(kc == 0), stop=(kc == KC - 1)),
                                reads=[w_b, hTb[tt]], writes=[pb[bank]], inc=(kc == KC - 1))
                        r0 = tok0 + tt * 128
                        if kind == "dt":
                            e_t, e_b, e_ch = ev32.next()
                            S.op("dve", lambda e_t=e_t, bank=bank: nc.vector.tensor_copy(out=e_t[:, 0:32], in_=ps[bank][:, 0:32]),
                                 reads=[pb[bank]], writes=[e_b])
                            S.dma("sp", lambda e_t=e_t, r0=r0: nc.sync.dma_start(out=dtr_d[r0:r0 + 128, :], in_=e_t[:, 0:32]),
                                  e_ch, reads=[e_b])
                        elif kind == "v":
                            o_t, o_b, o_ch = evb.next()
                            if tt % 2 == 0:
                                S.op("act", lambda o_t=o_t, bank=bank: nc.scalar.copy(out=o_t[:], in_=ps[bank][:]),
                                     reads=[pb[bank]], writes=[o_b])
                            else:
                                S.op("dve", lambda o_t=o_t, bank=bank: nc.vector.tensor_copy(out=o_t[:], in_=ps[bank][:]),
                                     reads=[pb[bank]], writes=[o_b])
                            S.dma("sp", lambda o_t=o_t, r0=r0, c0=c0: nc.sync.dma_start(
                                out=v_d[r0:r0 + 128, c0 - 4096:c0 - 4096 + 512], in_=o_t[:]), o_ch, reads=[o_b])
                        else:
                            o_t, o_b, o_ch = evb.next()
                            S.op("act", lambda o_t=o_t, bank=bank: nc.scalar.activation(out=o_t[:], in_=ps[bank][:], func=AF.Silu),
                                 reads=[pb[bank]], writes=[o_b])
                            S.dma("sp", lambda o_t=o_t, tt=tt, c0=c0: nc.sync.dma_start(
                                out=zs_d[tt * 128:(tt + 1) * 128, c0 - 6144:c0 - 6144 + 512], in_=o_t[:]), o_ch, reads=[o_b])
                        step_pending()
            step_pending(flush=True)
            S.barrier()

    NCV_CHUNKS = dbg.get("conv_chunks", 90) if need_moe else 0
    ncv_e = (NCV_CHUNKS + 5) // 6
    conv_state = {"next": 0}
    conv_bufs = {}
    if NCV_CHUNKS:
        wbg_d = dscr("s_wbg", [ncv_e, D, FF], BF16)
        wbu_d = dscr("s_wbu", [ncv_e, D, FF], BF16)
        wbd_d = dscr("s_wbd", [ncv_e, FF, D], BF16)
        conv_ch = S.chan()
        S.bg_chans.add(conv_ch)
        conv_order = [(e, k, h) for e in range(ncv_e) for (k, h) in (("g", 0), ("u", 0), ("g", 1), ("u", 1), ("d", 0), ("d", 1))][:NCV_CHUNKS]

    def bg_convert(n):
        for _ in range(n):
            i = conv_state["next"]
            if i >= NCV_CHUNKS:
                return
            conv_state["next"] = i + 1
            e, k, h = conv_order[i]
            if k == "d":
                src = w_down[e][:, h * 2048:(h + 1) * 2048].rearrange("(p r) c -> p r c", p=128)
                dst = wbd_d[e][:, h * 2048:(h + 1) * 2048].rearrange("(p r) c -> p r c", p=128)
            else:
                wsrc, wdst = (w_gate, wbg_d) if k == "g" else (w_up, wbu_d)
                src = wsrc[e][:, h * 512:(h + 1) * 512].rearrange("(p r) c -> p r c", p=128)
                dst = wdst[e][:, h * 512:(h + 1) * 512].rearrange("(p r) c -> p r c", p=128)
            b = Buf("conv%d" % i)
            conv_bufs[(e, k, h)] = b
            S.dma("pool", lambda src=src, dst=dst: nc.gpsimd.dma_start(out=dst, in_=src), conv_ch, writes=[b])

    def transpose_rows(src_t, src_b, dstT, dst_b, tt, evac_alt=0):
        for q4 in range(4):
            bank = q4
            pbf = ps[bank][:].bitcast(BF16)
            for j in range(8):
                kc = q4 * 8 + j
                S.op("pe", lambda pbf=pbf, j=j, kc=kc: nc.tensor.transpose(
                    out=pbf[:, j * 128:(j + 1) * 128], in_=src_t[:, kc * 128:(kc + 1) * 128], identity=ident_b[:]),
                    reads=[src_b, CB], writes=[pb[bank]], inc=(j == 7))
            src = pbf.rearrange("p (j t) -> p j t", j=8)
            dst = dstT[:, q4 * 8:(q4 + 1) * 8, tt * 128:(tt + 1) * 128]
            if (q4 + evac_alt) % 2 == 0:
                S.op("act", lambda dst=dst, src=src: nc.scalar.copy(out=dst, in_=src), reads=[pb[bank]], writes=[dst_b])
            else:
                S.op("dve", lambda dst=dst, src=src: nc.vector.tensor_copy(out=dst, in_=src), reads=[pb[bank]], writes=[dst_b])

    def attention():
        with ExitStack() as st:
            kTr = Rot(S, st, "kTh", [128, 2 * NTOK], BF16, 2, dma=True)
            qTr = Rot(S, st, "qTh", [128, NTOK], BF16, 2, dma=True)
            vr = Rot(S, st, "vh", [128, 16, 129], BF16, 2, dma=True)
            per = Rot(S, st, "pexp", [128, 512], F32, 2)
            ptr = Rot(S, st, "pT", [128, 512], BF16, 3)
            otr = Rot(S, st, "ot", [128, NT, 128], BF16, 2, dma=True)
            rcr = Rot(S, st, "rc", [128, 1], F32, 4)
            for i in range(2):
                S.op("dve", lambda i=i: nc.vector.memset(vr.t[i][:, NT:16, 128:129], 1.0), writes=[vr.b[i]])
                S.op("dve", lambda i=i: nc.vector.tensor_copy(out=vr.t[i][:, 0:NT, 128:129], in_=vflag[:, :].unsqueeze(2)),
                     reads=[CB], writes=[vr.b[i]])

            def loads(h):
                k_t, k_b, k_ch = kTr.next()
                q_t, q_b, q_ch = qTr.next()
                v_t, v_b, v_ch = vr.next()
                S.dma("sp", lambda: nc.sync.dma_start(out=k_t[:], in_=kT_d[h]), k_ch, writes=[k_b])
                S.dma("sp", lambda: nc.sync.dma_start(out=q_t[:], in_=qT_d[h]), q_ch, writes=[q_b])
                S.dma("sp", lambda: nc.sync.dma_start(
                    out=v_t[:, :, 0:128], in_=v_d[:, h * 128:(h + 1) * 128].rearrange("(t p) d -> p t d", p=128)),
                    v_ch, writes=[v_b])
                return (k_t, k_b, q_t, q_b, v_t, v_b)

            bg_convert(42)
            nxt = loads(0)
            heads = dbg.get("heads", NH)
            for h in range(heads):
                k_t, k_b, q_t, q_b, v_t, v_b = nxt
                if h + 1 < heads:
                    nxt = loads(h + 1)
                o_t, o_b, o_ch = otr.next()
                for c in range(2):
                    nkb = NT + 4 * c + 4

                    def s_mm(kb):
                        js = max(0, kb - NT - 4 * c)
                        bank = kb % 2
                        S.op("pe", lambda: nc.tensor.matmul(
                            ps[bank][:, js * 128:512], lhsT=k_t[:, kb * 128:(kb + 1) * 128],
                            rhs=q_t[:, c * 512 + js * 128:(c + 1) * 512], start=True, stop=True),
                            reads=[k_b, q_b], writes=[pb[bank]])
                    s_mm(0)
                    for kb in range(nkb):
                        if kb + 1 < nkb:
                            s_mm(kb + 1)
                        js = max(0, kb - NT - 4 * c)
                        bank = kb % 2
                        pe_t, pe_b = per.next()
                        S.op("act", lambda: nc.scalar.activation(out=pe_t[:, js * 128:512], in_=ps[bank][:, js * 128:512],
                                                                 func=AF.Exp), reads=[pb[bank]], writes=[pe_b])
                        pt_t, pt_b = ptr.next()
                        d0 = (NT + 4 * c + js) - kb
                        S.op("dve", lambda: nc.vector.tensor_tensor(
                            out=pt_t[:, js * 128:512], in0=pe_t[:, js * 128:512],
                            in1=amask[:, d0 * 128:(d0 + 4 - js) * 128], op=ALU.mult), reads=[pe_b, CB], writes=[pt_b])
                        for j in range(js, 4):
                            qb = NT + 4 * c + j
                            S.op("pe", lambda j=j, qb=qb: nc.tensor.matmul(
                                ps[2 + j][:, 0:129], lhsT=pt_t[:, j * 128:(j + 1) * 128], rhs=v_t[:, kb, :],
                                start=(kb == 0), stop=(kb == qb)), reads=[pt_b, v_b], writes=[pb[2 + j]])
                    for j in range(4):
                        rc_t, rc_b = rcr.next()
                        S.op("dve", lambda j=j, rc_t=rc_t: nc.vector.reciprocal(out=rc_t[:], in_=ps[2 + j][:, 128:129]),
                             reads=[pb[2 + j]], writes=[rc_b])
                        S.op("dve", lambda j=j, rc_t=rc_t: nc.vector.tensor_scalar(
                            out=o_t[:, 4 * c + j, :], in0=ps[2 + j][:, 0:128], scalar1=rc_t[:, 0:1], scalar2=None,
                            op0=ALU.mult), reads=[pb[2 + j], rc_b], writes=[o_b])
                S.dma("sp", lambda: nc.sync.dma_start(
                    out=mix_d[:, h * 128:(h + 1) * 128].rearrange("(t p) d -> p t d", p=128), in_=o_t[:]),
                    o_ch, reads=[o_b])
            S.barrier()

    def ssd():
        T2 = 2 * NTOK
        with ExitStack() as st:
            dt_all = sb(st, "dt_all", [128, 16, 32], F32)
            dA_all = sb(st, "dA_all", [128, 16, 32], F32)
            dtv = sb(st, "dtv", [128, 16, 32], F32)
            bbc = sb(st, "bbc", [128, 32], F32)
            abc = sb(st, "abc", [128, 32], F32)
            vcol = sb(st, "vcol", [128, 16], F32)
            cw = sb(st, "cw", [128, 32, 4], F32)
            cbias = sb(st, "cbias", [128, 32], F32)
            d_all = sb(st, "d_all", [128, 2048], F32)
            nw_all = sb(st, "nw_all", [128, 2048], F32)
            PB = Buf("ssd_params")
            pch = S.chan()
            S.dma("sp", lambda: nc.sync.dma_start(out=dt_all[:], in_=dtr_d.rearrange("(t p) h -> p t h", p=128)), pch, writes=[PB])
            S.dma("sp", lambda: nc.sync.dma_start(out=bbc[:], in_=dt_bias.partition_broadcast(128)), pch, writes=[PB])
            S.dma("sp", lambda: nc.sync.dma_start(out=abc[:], in_=a_log.partition_broadcast(128)), pch, writes=[PB])
            S.dma("sp", lambda: nc.sync.dma_start(out=cw[:], in_=conv_wc), pch, writes=[PB])
            S.dma("sp", lambda: nc.sync.dma_start(out=cbias[:], in_=conv_bc), pch, writes=[PB])
            S.dma("sp", lambda: nc.sync.dma_start(out=d_all[:], in_=d_rep.partition_broadcast(128)), pch, writes=[PB])
            S.dma("sp", lambda: nc.sync.dma_start(out=nw_all[:], in_=ssd_nw.partition_broadcast(128)), pch, writes=[PB])
            S.op("dve", lambda: nc.vector.tensor_tensor(out=dt_all[:], in0=dt_all[:],
                                                        in1=bbc[:, :].unsqueeze(1).to_broadcast([128, 16, 32]), op=ALU.add),
                 reads=[PB], writes=[PB])
            S.op("act", lambda: nc.scalar.activation(out=dt_all[:], in_=dt_all[:], func=AF.Exp), reads=[PB], writes=[PB])
            S.op("act", lambda: nc.scalar.activation(out=dt_all[:], in_=dt_all[:], func=AF.Ln, bias=ones_f[:, 0:1]),
                 reads=[PB, CB], writes=[PB])
            S.op("act", lambda: nc.scalar.activation(out=abc[:], in_=abc[:], func=AF.Exp), reads=[PB], writes=[PB])
            S.op("dve", lambda: nc.vector.scalar_tensor_tensor(
                out=dA_all[:], in0=dt_all[:], scalar=-1.0, in1=abc[:, :].unsqueeze(1).to_broadcast([128, 16, 32]),
                op0=ALU.mult, op1=ALU.mult), reads=[PB], writes=[PB])
            S.op("dve", lambda: nc.vector.memset(vcol[:, NT:16], 1.0), writes=[PB])
            S.op("dve", lambda: nc.vector.tensor_copy(out=vcol[:, 0:NT], in_=vflag[:, :]), reads=[CB], writes=[PB])
            S.op("dve", lambda: nc.vector.tensor_tensor(out=dtv[:], in0=dt_all[:],
                                                        in1=vcol[:, :].unsqueeze(2).to_broadcast([128, 16, 32]), op=ALU.mult),
                 reads=[PB], writes=[PB])

            lvl = dbg.get("ssd_level", 99)
            if lvl <= 1:
                S.barrier()
                return
            xraw = sb(st, "xraw", [128, 2, 3 + T2], F32)
            bcraw = sb(st, "bcraw", [128, 2, 3 + T2], F32)
            RAW = [Buf("raw%d" % i) for i in range(4)]
            rch = [S.chan() for _ in range(4)]
            S.op("dve", lambda: nc.vector.memset(xraw[:, :, 0:3], 0.0), writes=RAW[0:2])
            S.op("dve", lambda: nc.vector.memset(bcraw[:, :, 0:3], 0.0), writes=RAW[2:4])
            acc = Rot(S, st, "cacc", [128, T2], F32, 2)
            xTa = sb(st, "xTa", [128, 4, T2], BF16)
            XA = [Buf("xTa%d" % i) for i in range(4)]
            x_tok = sb(st, "x_tok", [128, 16, 256], BF16)
            b_tok = sb(st, "b_tok", [128, 16, 128], BF16)
            xdt = sb(st, "xdt", [128, 16, 256], BF16)
            XT = Buf("x_tok")
            BT = Buf("b_tok")
            XD = Buf("xdt")
            hst = sb(st, "hst", [128, 256], F32)
            hbf = sb(st, "hbf", [128, 256], BF16)
            HS = Buf("hst")
            HB = Buf("hbf")
            rhsr = Rot(S, st, "rhsH", [128, 128], F32, 3)
            segr = Rot(S, st, "segT", [128, 128], F32, 3)
            gtr = Rot(S, st, "GT", [128, 128], BF16, 3)
            dqr = Rot(S, st, "decq", [128, 4], F32, 2)
            csr = Rot(S, st, "ncs", [128, 64], F32, 2)
            cbr = Rot(S, st, "cbt", [128, 128], F32, 2)
            xwr = Rot(S, st, "xdtw", [128, 256], BF16, 2)
            ydr = Rot(S, st, "ydsb", [128, 256], F32, 2)
            yr = Rot(S, st, "yy", [128, 256], F32, 2)
            tmr = Rot(S, st, "ytmp", [128, 256], F32, 2)
            zr = Rot(S, st, "zst", [128, 256], BF16, 2, dma=True)
            yor = Rot(S, st, "yo", [128, 256], BF16, 2, dma=True)
            s2r = Rot(S, st, "ss2", [128, 2], F32, 2)
            jk = sb(st, "jk2", [128, 256], BF16)
            JK = Buf("jk2")
            groups = dbg.get("groups", 8)
            for g in range(groups):
                srcs = [(xraw, 0, g * 256, 2 * g), (xraw, 1, g * 256 + 128, 2 * g + 1),
                        (bcraw, 0, 2048 + g * 128, 16 + g), (bcraw, 1, 3072 + g * 128, 24 + g)]
                for i, (rt, ri, ch0, blk) in enumerate(srcs):
                    S.dma("sp", lambda rt=rt, ri=ri, ch0=ch0: nc.sync.dma_start(out=rt[:, ri, 3:3 + T2], in_=xbcT_d[ch0:ch0 + 128, :]),
                          rch[i], writes=[RAW[i]])
                for i, (rt, ri, ch0, blk) in enumerate(srcs):
                    a_t, a_b = acc.next()
                    S.op("dve", lambda rt=rt, ri=ri, blk=blk, a_t=a_t: nc.vector.tensor_scalar(
                        out=a_t[:], in0=rt[:, ri, 0:T2], scalar1=cw[:, blk, 0:1], scalar2=None, op0=ALU.mult),
                        reads=[RAW[i], PB], writes=[a_b])
                    for k in range(1, 4):
                        S.op("dve", lambda rt=rt, ri=ri, blk=blk, a_t=a_t, k=k: nc.vector.scalar_tensor_tensor(
                            out=a_t[:], in0=rt[:, ri, k:k + T2], scalar=cw[:, blk, k:k + 1], in1=a_t[:],
                            op0=ALU.mult, op1=ALU.add), reads=[RAW[i], PB, a_b], writes=[a_b])
                    S.op("act", lambda i=i, blk=blk, a_t=a_t: nc.scalar.activation(
                        out=xTa[:, i, :], in_=a_t[:], func=AF.Silu, bias=cbias[:, blk:blk + 1]),
                        reads=[a_b, PB], writes=[XA[i]])
                if lvl <= 2:
                    continue
                for t in range(16):
                    bank = 6 + t % 2
                    pbf = ps[bank][:].bitcast(BF16)
                    for i in range(3):
                        S.op("pe", lambda pbf=pbf, i=i, t=t: nc.tensor.transpose(
                            out=pbf[:, i * 128:(i + 1) * 128], in_=xTa[:, i, t * 128:(t + 1) * 128], identity=ident_b[:]),
                            reads=[XA[i], CB], writes=[pb[bank]], inc=(i == 2))
                    S.op("act", lambda pbf=pbf, t=t: nc.scalar.copy(out=x_tok[:, t, :], in_=pbf[:, 0:256]),
                         reads=[pb[bank]], writes=[XT])
                    S.op("dve", lambda pbf=pbf, t=t: nc.vector.tensor_copy(out=b_tok[:, t, :], in_=pbf[:, 256:384]),
                         reads=[pb[bank]], writes=[BT])
                S.op("dve", lambda g=g: nc.vector.tensor_tensor(
                    out=xdt[:, :, :].rearrange("p t (h d) -> p t h d", h=4),
                    in0=x_tok[:, :, :].rearrange("p t (h d) -> p t h d", h=4),
                    in1=dtv[:, :, 4 * g:4 * g + 4].unsqueeze(3).to_broadcast([128, 16, 4, 64]), op=ALU.mult),
                    reads=[XT, PB], writes=[XD])
                S.op("dve", lambda: nc.vector.memset(hst[:], 0.0), writes=[HS])
                S.op("dve", lambda: nc.vector.memset(hbf[:], 0.0), writes=[HB])
                if lvl <= 3:
                    continue
                for c in range(dbg.get("chunks", 16)):
                    own = c >= NT
                    cs = slice(c * 128, (c + 1) * 128)
                    S.op("pe", lambda c=c: nc.tensor.matmul(ps[2][:, 0:32], lhsT=tri_f[:], rhs=dA_all[:, c, :], start=True, stop=True),
                         reads=[CB, PB], writes=[pb[2]])
                    cs_t, cs_b = csr.next()
                    S.op("act", lambda cs_t=cs_t: nc.scalar.activation(out=cs_t[:, 0:32], in_=ps[2][:, 0:32], func=AF.Copy, scale=-1.0),
                         reads=[pb[2]], writes=[cs_b])
                    if own:
                        S.op("act", lambda cs_t=cs_t: nc.scalar.activation(out=cs_t[:, 32:64], in_=ps[2][:, 0:32], func=AF.Exp),
                             reads=[pb[2]], writes=[cs_b])
                        S.op("pe", lambda cs=cs: nc.tensor.matmul(ps[3][:, 0:128], lhsT=xTa[:, 2, cs], rhs=xTa[:, 3, cs], start=True, stop=True),
                             reads=[XA[2], XA[3]], writes=[pb[3]])
                        cb_t, cb_b = cbr.next()
                        S.op("dve", lambda cb_t=cb_t: nc.vector.tensor_copy(out=cb_t[:], in_=ps[3][:, 0:128]), reads=[pb[3]], writes=[cb_b])
                        S.op("pe", lambda cs=cs: nc.tensor.matmul(ps[4][:, 0:256], lhsT=xTa[:, 3, cs], rhs=hbf[:], start=True, stop=True),
                             reads=[XA[3], HB], writes=[pb[4]])
                    dq_t, dq_b = dqr.next()
                    xw_t, xw_b = xwr.next()
                    for hh in range(4):
                        H = 4 * g + hh
                        r_t, r_b = rhsr.next()
                        S.op("dve", lambda r_t=r_t, c=c, H=H: nc.vector.tensor_scalar(
                            out=r_t[:], in0=tri_f[:], scalar1=dA_all[:, c, H:H + 1], scalar2=None, op0=ALU.mult),
                            reads=[CB, PB], writes=[r_b])
                        bank = hh % 2
                        S.op("pe", lambda r_t=r_t, bank=bank: nc.tensor.matmul(ps[bank][:, 0:128], lhsT=ones_f[:], rhs=r_t[:], start=True, stop=False),
                             reads=[r_b, CB], writes=[pb[bank]], inc=False)
                        S.op("pe", lambda bank=bank: nc.tensor.matmul(ps[bank][:, 0:128], lhsT=ident_f[:], rhs=negm_f[:], start=False, stop=True),
                             reads=[CB], writes=[pb[bank]])
                        sg_t, sg_b = segr.next()
                        S.op("act", lambda sg_t=sg_t, bank=bank, cs_t=cs_t, H=H: nc.scalar.activation(
                            out=sg_t[:], in_=ps[bank][:, 0:128], func=AF.Exp, bias=cs_t[:, H:H + 1]),
                            reads=[pb[bank], cs_b], writes=[sg_b])
                        S.op("act", lambda dq_t=dq_t, bank=bank, hh=hh: nc.scalar.activation(
                            out=dq_t[:, hh:hh + 1], in_=ps[bank][:, 127:128], func=AF.Exp), reads=[pb[bank]], writes=[dq_b])
                        if own:
                            g_t, g_b = gtr.next()
                            S.op("dve", lambda g_t=g_t, cb_t=cb_t, sg_t=sg_t: nc.vector.tensor_tensor(
                                out=g_t[:], in0=cb_t[:], in1=sg_t[:], op=ALU.mult), reads=[cb_b, sg_b], writes=[g_b])
                            S.op("pe", lambda g_t=g_t, hh=hh, c=c: nc.tensor.matmul(
                                ps[5][:, hh * 64:(hh + 1) * 64], lhsT=g_t[:], rhs=xdt[:, c, hh * 64:(hh + 1) * 64], start=True, stop=True),
                                reads=[g_b, XD], writes=[pb[5]])
                        S.op("dve", lambda xw_t=xw_t, sg_t=sg_t, hh=hh, c=c: nc.vector.tensor_scalar(
                            out=xw_t[:, hh * 64:(hh + 1) * 64], in0=xdt[:, c, hh * 64:(hh + 1) * 64], scalar1=sg_t[:, 127:128],
                            scalar2=None, op0=ALU.mult), reads=[XD, sg_b], writes=[xw_b])
                    S.op("pe", lambda xw_t=xw_t, c=c: nc.tensor.matmul(ps[6][:, 0:256], lhsT=b_tok[:, c, :], rhs=xw_t[:], start=True, stop=True),
                         reads=[BT, xw_b], writes=[pb[6]])
                    if own:
                        yd_t, yd_b = ydr.next()
                        S.op("act", lambda yd_t=yd_t: nc.scalar.copy(out=yd_t[:], in_=ps[5][:, 0:256]), reads=[pb[5]], writes=[yd_b])
                        y_t, y_b = yr.next()
                        for hh in range(4):
                            H = 4 * g + hh
                            S.op("dve", lambda y_t=y_t, yd_t=yd_t, hh=hh, H=H, cs_t=cs_t: nc.vector.scalar_tensor_tensor(
                                out=y_t[:, hh * 64:(hh + 1) * 64], in0=ps[4][:, hh * 64:(hh + 1) * 64], scalar=cs_t[:, 32 + H:33 + H],
                                in1=yd_t[:, hh * 64:(hh + 1) * 64], op0=ALU.mult, op1=ALU.add),
                                reads=[pb[4], cs_b, yd_b], writes=[y_b])
                    for hh in range(4):
                        S.op("dve", lambda hh=hh, dq_t=dq_t: nc.vector.scalar_tensor_tensor(
                            out=hst[:, hh * 64:(hh + 1) * 64], in0=hst[:, hh * 64:(hh + 1) * 64], scalar=dq_t[:, hh:hh + 1],
                            in1=ps[6][:, hh * 64:(hh + 1) * 64], op0=ALU.mult, op1=ALU.add),
                            reads=[HS, dq_b, pb[6]], writes=[HS])
                    S.op("act", lambda: nc.scalar.copy(out=hbf[:], in_=hst[:]), reads=[HS], writes=[HB])
                    if own:
                        r0 = (c - NT) * 128
                        z_t, z_b, z_ch = zr.next()
                        S.dma("sp", lambda z_t=z_t, r0=r0, g=g: nc.sync.dma_start(out=z_t[:], in_=zs_d[r0:r0 + 128, g * 256:(g + 1) * 256]),
                              z_ch, writes=[z_b])
                        tm_t, tm_b = tmr.next()
                        S.op("dve", lambda tm_t=tm_t, c=c, g=g: nc.vector.tensor_tensor(
                            out=tm_t[:], in0=x_tok[:, c, :], in1=d_all[:, g * 256:(g + 1) * 256], op=ALU.mult),
                            reads=[XT, PB], writes=[tm_b])
                        S.op("dve", lambda tm_t=tm_t, y_t=y_t: nc.vector.tensor_tensor(out=y_t[:], in0=y_t[:], in1=tm_t[:], op=ALU.add),
                             reads=[tm_b, y_b], writes=[y_b])
                        S.op("dve", lambda z_t=z_t, y_t=y_t: nc.vector.tensor_tensor(out=y_t[:], in0=y_t[:], in1=z_t[:], op=ALU.mult),
                             reads=[z_b, y_b], writes=[y_b])
                        s_t, s_b = s2r.next()
                        S.op("act", lambda y_t=y_t, s_t=s_t: nc.scalar.activation(out=jk[:], in_=y_t[:], func=AF.Square, accum_out=s_t[:, 0:1]),
                             reads=[y_b], writes=[JK, s_b])
                        S.op("act", lambda s_t=s_t: nc.scalar.activation(out=s_t[:, 1:2], in_=s_t[:, 0:1], func=AF.Sqrt, scale=1.0 / 256,
                                                                         bias=eps5[:, 0:1]), reads=[s_b, CB], writes=[s_b])
                        S.op("dve", lambda s_t=s_t: nc.vector.reciprocal(out=s_t[:, 1:2], in_=s_t[:, 1:2]), reads=[s_b], writes=[s_b])
                        yo_t, yo_b, yo_ch = yor.next()
                        S.op("dve", lambda yo_t=yo_t, y_t=y_t, s_t=s_t, g=g: nc.vector.scalar_tensor_tensor(
                            out=yo_t[:], in0=y_t[:], scalar=s_t[:, 1:2], in1=nw_all[:, g * 256:(g + 1) * 256],
                            op0=ALU.mult, op1=ALU.mult), reads=[y_b, s_b, PB], writes=[yo_b])
                        S.dma("sp", lambda yo_t=yo_t, r0=r0, g=g: nc.sync.dma_start(
                            out=mix_d[r0:r0 + 128, 2048 + g * 256:2048 + (g + 1) * 256], in_=yo_t[:]), yo_ch, reads=[yo_b])
            S.barrier()

    def ssd2():
        T2 = 2 * NTOK
        with ExitStack() as st:
            dt_all = sb(st, "dt_all", [128, 16, 32], F32)
            dA_all = sb(st, "dA_all", [128, 16, 32], F32)
            dtv = sb(st, "dtv", [128, 16, 32], F32)
            ncs_all = sb(st, "ncs_all", [128, 16, 32], F32)
            ecs_all = sb(st, "ecs_all", [128, 16, 32], F32)
            decq_all = sb(st, "decq_all", [128, 16, 32], F32)
            w2_all = sb(st, "w2_all", [128, 16, 32], F32)
            bbc = sb(st, "bbc", [128, 32], F32)
            abc = sb(st, "abc", [128, 32], F32)
            vcol = sb(st, "vcol", [128, 16], F32)
            cw = sb(st, "cw", [128, 32, 4], F32)
            cbias = sb(st, "cbias", [128, 32], F32)
            d_all = sb(st, "d_all", [128, 2048], F32)
            nw_all = sb(st, "nw_all", [128, 2048], F32)
            negm4 = sb(st, "negm4", [128, 4, 128], F32)
            PB = Buf("ssd_params")
            pch = S.chan()
            S.dma("sp", lambda: nc.sync.dma_start(out=dt_all[:], in_=dtr_d.rearrange("(t p) h -> p t h", p=128)), pch, writes=[PB])
            S.dma("sp", lambda: nc.sync.dma_start(out=bbc[:], in_=dt_bias.partition_broadcast(128)), pch, writes=[PB])
            S.dma("sp", lambda: nc.sync.dma_start(out=abc[:], in_=a_log.partition_broadcast(128)), pch, writes=[PB])
            S.dma("sp", lambda: nc.sync.dma_start(out=cw[:], in_=conv_wc), pch, writes=[PB])
            S.dma("sp", lambda: nc.sync.dma_start(out=cbias[:], in_=conv_bc), pch, writes=[PB])
            S.dma("sp", lambda: nc.sync.dma_start(out=d_all[:], in_=d_rep.partition_broadcast(128)), pch, writes=[PB])
            S.dma("sp", lambda: nc.sync.dma_start(out=nw_all[:], in_=ssd_nw.partition_broadcast(128)), pch, writes=[PB])
            S.op("dve", lambda: nc.vector.tensor_tensor(out=dt_all[:], in0=dt_all[:],
                                                        in1=bbc[:, :].unsqueeze(1).to_broadcast([128, 16, 32]), op=ALU.add),
                 reads=[PB], writes=[PB])
            S.op("act", lambda: nc.scalar.activation(out=dt_all[:], in_=dt_all[:], func=AF.Exp), reads=[PB], writes=[PB])
            S.op("act", lambda: nc.scalar.activation(out=dt_all[:], in_=dt_all[:], func=AF.Ln, bias=ones_f[:, 0:1]),
                 reads=[PB, CB], writes=[PB])
            S.op("act", lambda: nc.scalar.activation(out=abc[:], in_=abc[:], func=AF.Exp), reads=[PB], writes=[PB])
            S.op("dve", lambda: nc.vector.scalar_tensor_tensor(
                out=dA_all[:], in0=dt_all[:], scalar=-1.0, in1=abc[:, :].unsqueeze(1).to_broadcast([128, 16, 32]),
                op0=ALU.mult, op1=ALU.mult), reads=[PB], writes=[PB])
            S.op("dve", lambda: nc.vector.memset(vcol[:, NT:16], 1.0), writes=[PB])
            S.op("dve", lambda: nc.vector.tensor_copy(out=vcol[:, 0:NT], in_=vflag[:, :]), reads=[CB], writes=[PB])
            S.op("dve", lambda: nc.vector.tensor_tensor(out=dtv[:], in0=dt_all[:],
                                                        in1=vcol[:, :].unsqueeze(2).to_broadcast([128, 16, 32]), op=ALU.mult),
                 reads=[PB], writes=[PB])
            S.op("dve", lambda: nc.vector.tensor_copy(out=negm4[:], in_=negm_f[:, :].unsqueeze(1).to_broadcast([128, 4, 128])),
                 reads=[CB], writes=[PB])
            flat = lambda t: t[:, :, :].rearrange("p c h -> p (c h)")
            S.op("pe", lambda: nc.tensor.matmul(ps[2][:], lhsT=tri_f[:], rhs=flat(dA_all), start=True, stop=True),
                 reads=[CB, PB], writes=[pb[2]])
            S.op("pe", lambda: nc.tensor.matmul(ps[3][:], lhsT=ones_f[:], rhs=flat(dA_all), start=True, stop=True),
                 reads=[CB, PB], writes=[pb[3]])
            S.op("act", lambda: nc.scalar.activation(out=flat(ncs_all), in_=ps[2][:], func=AF.Copy, scale=-1.0),
                 reads=[pb[2]], writes=[PB])
            S.op("act", lambda: nc.scalar.activation(out=flat(ecs_all), in_=ps[2][:], func=AF.Exp), reads=[pb[2]], writes=[PB])
            S.op("act", lambda: nc.scalar.activation(out=flat(decq_all), in_=ps[3][:], func=AF.Exp), reads=[pb[3]], writes=[PB])
            S.op("dve", lambda: nc.vector.tensor_tensor(out=flat(w2_all), in0=ps[3][:], in1=flat(ncs_all), op=ALU.add),
                 reads=[pb[3], PB], writes=[PB])
            S.op("act", lambda: nc.scalar.activation(out=flat(w2_all), in_=flat(w2_all), func=AF.Exp), reads=[PB], writes=[PB])
            S.op("dve", lambda: nc.vector.tensor_tensor(out=flat(w2_all), in0=flat(w2_all), in1=flat(dtv), op=ALU.mult),
                 reads=[PB], writes=[PB])

            xraw = sb(st, "xraw", [128, 2, 3 + T2], F32)
            bcraw = sb(st, "bcraw", [128, 2, 3 + T2], F32)
            RAW = [Buf("raw%d" % i) for i in range(4)]
            rch = [S.chan() for _ in range(4)]
            S.op("dve", lambda: nc.vector.memset(xraw[:, :, 0:3], 0.0), writes=RAW[0:2])
            S.op("dve", lambda: nc.vector.memset(bcraw[:, :, 0:3], 0.0), writes=RAW[2:4])
            acc = Rot(S, st, "cacc", [128, T2], F32, 2)
            xTa = sb(st, "xTa", [128, 4, T2], BF16)
            XA = [Buf("xTa%d" % i) for i in range(4)]
            x_tok = sb(st, "x_tok", [128, 16, 256], BF16)
            b_tok = sb(st, "b_tok", [128, 16, 128], BF16)
            xdt = sb(st, "xdt", [128, NT, 256], BF16)
            xdtw = sb(st, "xdtw_all", [128, 16, 256], BF16)
            XT = Buf("x_tok")
            BT = Buf("b_tok")
            XD = Buf("xdt")
            XW = Buf("xdtw")
            S_all = sb(st, "S_all", [128, 16, 256], F32)
            SB_ = [Buf("S%d" % i) for i in range(16)]
            hbf_all = sb(st, "hbf_all", [128, NT, 256], BF16)
            HB = Buf("hbf_all")
            htmp = Rot(S, st, "htmp", [128, 256], F32, 2)
            y_all = sb(st, "y_all", [128, NT, 256], F32)
            YB = [Buf("y%d" % i) for i in range(NT)]
            dx_all = sb(st, "dx_all", [128, NT, 256], F32)
            DX = Buf("dx_all")
            zs_g = sb(st, "zs_g", [128, NT, 256], BF16)
            ZG = Buf("zs_g")
            zch = S.chan()
            yo_g = sb(st, "yo_g", [128, NT, 256], BF16)
            YO = Buf("yo_g")
            ych = S.chan()
            ssg = sb(st, "ssg", [128, 2 * NT], F32)
            SG = Buf("ssg")
            rhsr = Rot(S, st, "rhsH4", [128, 4, 128], F32, 2)
            segr = Rot(S, st, "segT4", [128, 4, 128], F32, 2)
            gtr = Rot(S, st, "GT4", [128, 4, 128], BF16, 2)
            cbr = Rot(S, st, "cbt", [128, 128], F32, 2)
            ydr = Rot(S, st, "ydsb", [128, 256], F32, 2)
            groups = dbg.get("groups", 8)
            for g in range(groups):
                hs4 = slice(4 * g, 4 * g + 4)
                srcs = [(xraw, 0, g * 256, 2 * g), (xraw, 1, g * 256 + 128, 2 * g + 1),
                        (bcraw, 0, 2048 + g * 128, 16 + g), (bcraw, 1, 3072 + g * 128, 24 + g)]
                for i, (rt, ri, ch0, blk) in enumerate(srcs):
                    S.dma("sp", lambda rt=rt, ri=ri, ch0=ch0: nc.sync.dma_start(out=rt[:, ri, 3:3 + T2], in_=xbcT_d[ch0:ch0 + 128, :]),
                          rch[i], writes=[RAW[i]])
                S.dma("sp", lambda g=g: nc.sync.dma_start(
                    out=zs_g[:], in_=zs_d[:, g * 256:(g + 1) * 256].rearrange("(t p) d -> p t d", p=128)), zch, writes=[ZG])
                for i, (rt, ri, ch0, blk) in enumerate(srcs):
                    a_t, a_b = acc.next()
                    E = "dve"
                    eng = nc.vector
                    S.op(E, lambda rt=rt, ri=ri, blk=blk, a_t=a_t, eng=eng: eng.tensor_scalar(
                        out=a_t[:], in0=rt[:, ri, 0:T2], scalar1=cw[:, blk, 0:1], scalar2=None, op0=ALU.mult),
                        reads=[RAW[i], PB], writes=[a_b])
                    for k in range(1, 4):
                        if E == "dve":
                            S.op(E, lambda rt=rt, ri=ri, blk=blk, a_t=a_t, k=k, eng=eng: eng.scalar_tensor_tensor(
                                out=a_t[:], in0=rt[:, ri, k:k + T2], scalar=cw[:, blk, k:k + 1], in1=a_t[:],
                                op0=ALU.mult, op1=ALU.add), reads=[RAW[i], PB, a_b], writes=[a_b])
                        else:
                            tmpv = dx_all[:, :, :].rearrange("p t d -> p (t d)")
                            S.op(E, lambda rt=rt, ri=ri, blk=blk, k=k, tmpv=tmpv: nc.gpsimd.tensor_scalar(
                                out=tmpv, in0=rt[:, ri, k:k + T2], scalar1=cw[:, blk, k:k + 1], scalar2=None, op0=ALU.mult),
                                reads=[RAW[i], PB], writes=[DX])
                            S.op(E, lambda a_t=a_t, tmpv=tmpv: nc.gpsimd.tensor_tensor(out=a_t[:], in0=a_t[:], in1=tmpv, op=ALU.add),
                                 reads=[DX, a_b], writes=[a_b])
                    S.op("act", lambda i=i, blk=blk, a_t=a_t: nc.scalar.activation(
                        out=xTa[:, i, :], in_=a_t[:], func=AF.Silu, bias=cbias[:, blk:blk + 1]),
                        reads=[a_b, PB], writes=[XA[i]])
                for t in range(16):
                    bank = 6 + t % 2
                    pbf = ps[bank][:].bitcast(BF16)
                    for i in range(3):
                        S.op("pe", lambda pbf=pbf, i=i, t=t: nc.tensor.transpose(
                            out=pbf[:, i * 128:(i + 1) * 128], in_=xTa[:, i, t * 128:(t + 1) * 128], identity=ident_b[:]),
                            reads=[XA[i], CB], writes=[pb[bank]], inc=(i == 2))
                    S.op("act", lambda pbf=pbf, t=t: nc.scalar.copy(out=x_tok[:, t, :], in_=pbf[:, 0:256]),
                         reads=[pb[bank]], writes=[XT])
                    S.op("act", lambda pbf=pbf, t=t: nc.scalar.copy(out=b_tok[:, t, :], in_=pbf[:, 256:384]),
                         reads=[pb[bank]], writes=[BT])
                v4 = lambda ap: ap.rearrange("p t (h d) -> p t h d", h=4)
                S.op("dve", lambda: nc.vector.tensor_tensor(
                    out=v4(xdtw[:, :, :]), in0=v4(x_tok[:, :, :]),
                    in1=w2_all[:, :, hs4].unsqueeze(3).to_broadcast([128, 16, 4, 64]), op=ALU.mult),
                    reads=[XT, PB], writes=[XW])
                S.op("dve", lambda: nc.vector.tensor_tensor(
                    out=v4(xdt[:, :, :]), in0=v4(x_tok[:, NT:16, :]),
                    in1=dtv[:, NT:16, hs4].unsqueeze(3).to_broadcast([128, NT, 4, 64]), op=ALU.mult),
                    reads=[XT, PB], writes=[XD])
                S.op("dve", lambda g=g: nc.vector.tensor_tensor(
                    out=dx_all[:, :, :], in0=x_tok[:, NT:16, :],
                    in1=d_all[:, g * 256:(g + 1) * 256].unsqueeze(1).to_broadcast([128, NT, 256]), op=ALU.mult),
                    reads=[XT, PB], writes=[DX])
                for c2 in range(8):
                    bank = 6 + c2 % 2
                    ncs_ = 2 if c2 < 7 else 1
                    for u in range(ncs_):
                        c = 2 * c2 + u
                        S.op("pe", lambda c=c, u=u, bank=bank: nc.tensor.matmul(
                            ps[bank][:, u * 256:(u + 1) * 256], lhsT=b_tok[:, c, :], rhs=xdtw[:, c, :], start=True, stop=True),
                            reads=[BT, XW], writes=[pb[bank]])
                    dst = S_all[:, 2 * c2:2 * c2 + ncs_, :]
                    src = ps[bank][:, 0:256 * ncs_].rearrange("p (u d) -> p u d", u=ncs_)
                    if c2 % 2 == 0:
                        S.op("act", lambda dst=dst, src=src: nc.scalar.copy(out=dst, in_=src), reads=[pb[bank]],
                             writes=SB_[2 * c2:2 * c2 + ncs_])
                    else:
                        S.op("dve", lambda dst=dst, src=src: nc.vector.tensor_copy(out=dst, in_=src), reads=[pb[bank]],
                             writes=SB_[2 * c2:2 * c2 + ncs_])
                for c in range(1, 15):
                    h_t, h_b = htmp.next()
                    S.op("dve", lambda c=c, h_t=h_t: nc.vector.tensor_tensor(
                        out=h_t[:, :].rearrange("p (h d) -> p h d", h=4),
                        in0=S_all[:, c - 1, :].rearrange("p (h d) -> p h d", h=4),
                        in1=decq_all[:, c, hs4].unsqueeze(2).to_broadcast([128, 4, 64]), op=ALU.mult),
                        reads=[SB_[c - 1], PB], writes=[h_b])
                    S.op("dve", lambda c=c, h_t=h_t: nc.vector.tensor_tensor(out=S_all[:, c, :], in0=S_all[:, c, :], in1=h_t[:], op=ALU.add),
                         reads=[h_b, SB_[c]], writes=[SB_[c]])
                S.op("act", lambda: nc.scalar.copy(out=hbf_all[:, :, :], in_=S_all[:, NT - 1:2 * NT - 1, :]),
                     reads=SB_[NT - 1:2 * NT - 1], writes=[HB])
                for j in range(NT):
                    c = NT + j
                    cs = slice(c * 128, (c + 1) * 128)
                    r_t, r_b = rhsr.next()
                    S.op("dve", lambda r_t=r_t, c=c: nc.vector.tensor_tensor(
                        out=r_t[:, :, :], in0=tri_f[:, :].unsqueeze(1).to_broadcast([128, 4, 128]),
                        in1=dA_all[:, c, hs4].unsqueeze(2).to_broadcast([128, 4, 128]), op=ALU.mult),
                        reads=[CB, PB], writes=[r_b])
                    bank = j % 2
                    S.op("pe", lambda r_t=r_t, bank=bank: nc.tensor.matmul(
                        ps[bank][:], lhsT=ones_f[:], rhs=r_t[:, :, :].rearrange("p h l -> p (h l)"), start=True, stop=False),
                        reads=[r_b, CB], writes=[pb[bank]], inc=False)
                    S.op("pe", lambda bank=bank: nc.tensor.matmul(
                        ps[bank][:], lhsT=ident_f[:], rhs=negm4[:, :, :].rearrange("p h l -> p (h l)"), start=False, stop=True),
                        reads=[CB, PB], writes=[pb[bank]])
                    sg_t, sg_b = segr.next()
                    for hh in range(4):
                        S.op("act", lambda sg_t=sg_t, bank=bank, hh=hh, c=c, g=g: nc.scalar.activation(
                            out=sg_t[:, hh, :], in_=ps[bank][:, hh * 128:(hh + 1) * 128], func=AF.Exp,
                            bias=ncs_all[:, c, 4 * g + hh:4 * g + hh + 1]), reads=[pb[bank], PB], writes=[sg_b])
                    S.op("pe", lambda cs=cs: nc.tensor.matmul(ps[3][:, 0:128], lhsT=xTa[:, 2, cs], rhs=xTa[:, 3, cs], start=True, stop=True),
                         reads=[XA[2], XA[3]], writes=[pb[3]])
                    cb_t, cb_b = cbr.next()
                    S.op("act", lambda cb_t=cb_t: nc.scalar.copy(out=cb_t[:], in_=ps[3][:, 0:128]), reads=[pb[3]], writes=[cb_b])
                    g_t, g_b = gtr.next()
                    S.op("dve", lambda g_t=g_t, cb_t=cb_t, sg_t=sg_t: nc.vector.tensor_tensor(
                        out=g_t[:, :, :], in0=sg_t[:, :, :], in1=cb_t[:, :].unsqueeze(1).to_broadcast([128, 4, 128]), op=ALU.mult),
                        reads=[cb_b, sg_b], writes=[g_b])
                    for hh in range(4):
                        S.op("pe", lambda g_t=g_t, hh=hh, j=j: nc.tensor.matmul(
                            ps[5][:, hh * 64:(hh + 1) * 64], lhsT=g_t[:, hh, :], rhs=xdt[:, j, hh * 64:(hh + 1) * 64], start=True, stop=True),
                            reads=[g_b, XD], writes=[pb[5]], inc=(hh == 3))
                    S.op("pe", lambda cs=cs, j=j: nc.tensor.matmul(ps[4][:, 0:256], lhsT=xTa[:, 3, cs], rhs=hbf_all[:, j, :], start=True, stop=True),
                         reads=[XA[3], HB], writes=[pb[4]])
                    yd_t, yd_b = ydr.next()
                    S.op("act", lambda yd_t=yd_t: nc.scalar.copy(out=yd_t[:], in_=ps[5][:, 0:256]), reads=[pb[5]], writes=[yd_b])
                    S.op("dve", lambda j=j, c=c: nc.vector.tensor_tensor(
                        out=y_all[:, j, :].rearrange("p (h d) -> p h d", h=4),
                        in0=ps[4][:, 0:256].rearrange("p (h d) -> p h d", h=4),
                        in1=ecs_all[:, c, hs4].unsqueeze(2).to_broadcast([128, 4, 64]), op=ALU.mult),
                        reads=[pb[4], PB], writes=[YB[j]])
                    S.op("dve", lambda j=j, yd_t=yd_t: nc.vector.tensor_tensor(out=y_all[:, j, :], in0=y_all[:, j, :], in1=yd_t[:], op=ALU.add),
                         reads=[yd_b, YB[j]], writes=[YB[j]])
                S.op("dve", lambda: nc.vector.tensor_tensor(out=y_all[:, :, :], in0=y_all[:, :, :], in1=dx_all[:, :, :], op=ALU.add),
                     reads=YB + [DX], writes=YB)
                S.op("dve", lambda: nc.vector.tensor_tensor(out=y_all[:, :, :], in0=y_all[:, :, :], in1=zs_g[:, :, :], op=ALU.mult),
                     reads=YB + [ZG], writes=YB)
                S.op("act", lambda: nc.scalar.activation(out=dx_all[:, :, :], in_=y_all[:, :, :], func=AF.Square),
                     reads=YB, writes=[DX])
                S.op("dve", lambda: nc.vector.tensor_reduce(out=ssg[:, 0:NT], in_=dx_all[:, :, :], axis=AX.X, op=ALU.add),
                     reads=[DX], writes=[SG])
                S.op("act", lambda: nc.scalar.activation(out=ssg[:, NT:2 * NT], in_=ssg[:, 0:NT], func=AF.Sqrt, scale=1.0 / 256,
                                                         bias=eps5[:, 0:1]), reads=[SG, CB], writes=[SG])
                S.op("dve", lambda: nc.vector.reciprocal(out=ssg[:, NT:2 * NT], in_=ssg[:, NT:2 * NT]), reads=[SG], writes=[SG])
                S.op("dve", lambda: nc.vector.tensor_tensor(
                    out=y_all[:, :, :], in0=y_all[:, :, :], in1=ssg[:, NT:2 * NT].unsqueeze(2).to_broadcast([128, NT, 256]), op=ALU.mult),
                    reads=YB + [SG], writes=YB)
                S.op("dve", lambda g=g: nc.vector.tensor_tensor(
                    out=yo_g[:, :, :], in0=y_all[:, :, :],
                    in1=nw_all[:, g * 256:(g + 1) * 256].unsqueeze(1).to_broadcast([128, NT, 256]), op=ALU.mult),
                    reads=YB + [PB], writes=[YO])
                S.dma("sp", lambda g=g: nc.sync.dma_start(
                    out=mix_d[:, 2048 + g * 256:2048 + (g + 1) * 256].rearrange("(t p) d -> p t d", p=128), in_=yo_g[:]),
                    ych, reads=[YO])
            S.barrier()

    if not dbg.get("skip_proj"):
        proj_pass(x_pre, pos_pre, False)
    if stop not in ("A", "Bpre") and not dbg.get("skip_proj"):
        proj_pass(x_own, pos_own, True)
    if stop not in ("A", "Bpre", "B"):
        if not dbg.get("skip_attn"):
            attention()
        if not dbg.get("skip_ssd"):
            ssd2() if not dbg.get("old_ssd") else ssd()

    ssp = sb(es, "ssp", [128, NT, 8], F32)
    SSP = Buf("ssp")
    gate_all = sb(es, "gate_all", [128, NT, 2], F32)
    dest_all = sb(es, "dest_all", [128, NT, 2], I32)
    GD = Buf("gates_dest")

    def wout():
        with ExitStack() as st:
            mixT = sb(st, "mixT", [128, KC, NTOK], BF16)
            mTb = [Buf("mixT%d" % i) for i in range(NT)]
            with ExitStack() as sa:
                mr = Rot(S, sa, "mixrow", [128, D], BF16, 2, dma=True)
                for tt in range(NT):
                    m_t, m_b, m_ch = mr.next()
                    S.dma("sp", lambda m_t=m_t, tt=tt: nc.sync.dma_start(out=m_t[:], in_=mix_d[tt * 128:(tt + 1) * 128, :]),
                          m_ch, writes=[m_b])
                    transpose_rows(m_t, m_b, mixT, mTb[tt], tt)
                S.barrier()
            wv = w_out.rearrange("(kc p) n -> p kc n", p=128)
            wt = Rot(S, st, "wot", [128, KC, 512], BF16, 2, dma=True)
            xr = Rot(S, st, "xres", [128, 512], F32, 3, dma=True)
            x2r = Rot(S, st, "x2t", [128, 512], F32, 3, dma=True)
            junk2 = sb(st, "junk3", [128, 512], BF16)
            J2 = Buf("junk3")
            nbank = 0
            for cb in range(8):
                w_t, w_b, w_ch = wt.next()
                S.dma("pool", lambda w_t=w_t, cb=cb: nc.gpsimd.dma_start(out=w_t[:], in_=wv[:, :, cb * 512:(cb + 1) * 512]),
                      w_ch, writes=[w_b])
                for tt in range(NT):
                    x_t, x_b, x_ch = xr.next()
                    S.dma("sp", lambda x_t=x_t, tt=tt, cb=cb: nc.sync.dma_start(
                        out=x_t[:], in_=x_own[tt * 128:(tt + 1) * 128, cb * 512:(cb + 1) * 512]), x_ch, writes=[x_b])
                    bank = nbank % 4
                    nbank += 1
                    for kc in range(KC):
                        S.op("pe", lambda bank=bank, kc=kc, tt=tt, w_t=w_t: nc.tensor.matmul(
                            ps[bank][:], lhsT=mixT[:, kc, tt * 128:(tt + 1) * 128], rhs=w_t[:, kc, :],
                            start=(kc == 0), stop=(kc == KC - 1)), reads=[w_b, mTb[tt]], writes=[pb[bank]], inc=(kc == KC - 1))
                    o_t, o_b, o_ch = x2r.next()
                    S.op("dve", lambda o_t=o_t, bank=bank, x_t=x_t: nc.vector.tensor_tensor(out=o_t[:], in0=ps[bank][:], in1=x_t[:], op=ALU.add),
                         reads=[pb[bank], x_b], writes=[o_b])
                    S.op("act", lambda o_t=o_t, tt=tt, cb=cb: nc.scalar.activation(out=junk2[:], in_=o_t[:], func=AF.Square,
                                                                                  accum_out=ssp[:, tt, cb:cb + 1]),
                         reads=[o_b], writes=[J2, SSP])
                    S.dma("sp", lambda o_t=o_t, tt=tt, cb=cb: nc.sync.dma_start(
                        out=x2_d[tt * 128:(tt + 1) * 128, cb * 512:(cb + 1) * 512], in_=o_t[:]), o_ch, reads=[o_b])
            S.barrier()

    def route():
        with ExitStack() as st:
            wr = sb(st, "wr", [128, KC, 36], F32)
            nfc = sb(st, "nfc", [128, KC], F32)
            brb = sb(st, "brb", [128, 36], F32)
            ebase = sb(st, "ebase", [128, NE], F32)
            wbc2 = sb(st, "wbc2", [128, D], F32)
            RP = Buf("route_params")
            rpc = S.chan()
            S.dma("sp", lambda: nc.sync.dma_start(out=wr[:], in_=w_r), rpc, writes=[RP])
            S.dma("sp", lambda: nc.sync.dma_start(out=nfc[:], in_=nfw_col), rpc, writes=[RP])
            S.dma("sp", lambda: nc.sync.dma_start(out=brb[:], in_=b_r.partition_broadcast(128)), rpc, writes=[RP])
            S.dma("sp", lambda: nc.sync.dma_start(out=ebase[:], in_=c_ebase), rpc, writes=[RP])
            S.dma("sp", lambda: nc.sync.dma_start(out=wbc2[:], in_=norm_ffn_w.partition_broadcast(128)), rpc, writes=[RP])
            S.op("dve", lambda: nc.vector.tensor_tensor(out=wr[:], in0=wr[:], in1=nfc[:, :].unsqueeze(2).to_broadcast([128, KC, 36]),
                                                        op=ALU.mult), reads=[RP], writes=[RP])
            bg_convert(NCV_CHUNKS)
            x2r = Rot(S, st, "x2row", [128, D], F32, 2, dma=True)
            x2T = sb(st, "x2T", [128, KC, 128], F32)
            XTB = Buf("x2T")
            h2r = Rot(S, st, "h2b", [128, D], BF16, 2, dma=True)
            oh_all = sb(st, "oh_all", [128, NT, NE], BF16)
            OHB = [Buf("oh%d" % i) for i in range(NT)]
            sm = Rot(S, st, "rsm", [128, 160], F32, 2)
            for tt in range(NT):
                x_t, x_b, x_ch = x2r.next()
                S.dma("sp", lambda x_t=x_t, tt=tt: nc.sync.dma_start(out=x_t[:], in_=x2_d[tt * 128:(tt + 1) * 128, :]), x_ch, writes=[x_b])
                t_, tb = sm.next()
                V = lambda a, b: t_[:, a:b]
                dv = lambda fn, rd=(), wr_=None: S.op("dve", fn, reads=[tb] + list(rd), writes=[tb] + list(wr_ or []))
                dv(lambda: nc.vector.tensor_reduce(out=V(0, 1), in_=ssp[:, tt, :], axis=AX.X, op=ALU.add), rd=[SSP])
                S.op("act", lambda: nc.scalar.activation(out=V(1, 2), in_=V(0, 1), func=AF.Sqrt, scale=1.0 / D, bias=eps6[:, 0:1]),
                     reads=[tb, CB], writes=[tb])
                dv(lambda: nc.vector.reciprocal(out=V(1, 2), in_=V(1, 2)))
                h_t, h_b, h_ch = h2r.next()
                S.op("dve", lambda: nc.vector.scalar_tensor_tensor(out=h_t[:], in0=x_t[:], scalar=V(1, 2), in1=wbc2[:],
                                                                   op0=ALU.mult, op1=ALU.mult), reads=[x_b, tb, RP], writes=[h_b])
                for q8 in range(8):
                    bank = q8 % 4
                    for j in range(4):
                        kc = q8 * 4 + j
                        S.op("pe", lambda bank=bank, j=j, kc=kc: nc.tensor.transpose(
                            out=ps[bank][:, j * 128:(j + 1) * 128], in_=x_t[:, kc * 128:(kc + 1) * 128], identity=ident_f[:]),
                            reads=[x_b, CB], writes=[pb[bank]], inc=(j == 3))
                    src = ps[bank][:].rearrange("p (j t) -> p j t", j=4)
                    dst = x2T[:, q8 * 4:(q8 + 1) * 4, :]
                    if q8 % 2 == 0:
                        S.op("act", lambda dst=dst, src=src: nc.scalar.copy(out=dst, in_=src), reads=[pb[bank]], writes=[XTB])
                    else:
                        S.op("dve", lambda dst=dst, src=src: nc.vector.tensor_copy(out=dst, in_=src), reads=[pb[bank]], writes=[XTB])
                for kc in range(KC):
                    S.op("pe", lambda kc=kc: nc.tensor.matmul(ps[4][:, 0:36], lhsT=x2T[:, kc, :], rhs=wr[:, kc, :],
                                                              start=(kc == 0), stop=(kc == KC - 1)),
                         reads=[XTB, RP], writes=[pb[4]], inc=(kc == KC - 1))
                dv(lambda: nc.vector.scalar_tensor_tensor(out=V(4, 40), in0=ps[4][:, 0:36], scalar=V(1, 2), in1=brb[:],
                                                          op0=ALU.mult, op1=ALU.add), rd=[pb[4], RP])
                LG = (4, 8)
                LE = (8, 40)
                dv(lambda: nc.vector.tensor_reduce(out=V(2, 3), in_=V(*LG), axis=AX.X, op=ALU.max))
                dv(lambda: nc.vector.tensor_scalar(out=V(40, 44), in0=V(*LG), scalar1=V(2, 3), scalar2=None, op0=ALU.is_equal))
                dv(lambda: nc.vector.tensor_scalar(out=V(3, 4), in0=V(2, 3), scalar1=-1.0, scalar2=None, op0=ALU.mult))
                S.op("act", lambda: nc.scalar.activation(out=V(44, 48), in_=V(*LG), func=AF.Exp, bias=V(3, 4), accum_out=V(48, 49)),
                     reads=[tb], writes=[tb])
                dv(lambda: nc.vector.reciprocal(out=V(49, 50), in_=V(48, 49)))
                dv(lambda: nc.vector.tensor_scalar(out=V(50, 54), in0=V(40, 44), scalar1=-1.0, scalar2=1e30, op0=ALU.add, op1=ALU.mult))
                dv(lambda: nc.vector.tensor_tensor(out=V(56, 88).rearrange("p (g e) -> p g e", g=4),
                                                   in0=V(*LE).rearrange("p (g e) -> p g e", g=4),
                                                   in1=V(50, 54).unsqueeze(2).to_broadcast([128, 4, 8]), op=ALU.add))
                dv(lambda: nc.vector.tensor_reduce(out=V(54, 55), in_=V(56, 88), axis=AX.X, op=ALU.max))
                dv(lambda: nc.vector.tensor_scalar(out=V(88, 120), in0=V(56, 88), scalar1=V(54, 55), scalar2=None, op0=ALU.is_equal))
                dv(lambda: nc.vector.scalar_tensor_tensor(out=V(56, 88), in0=V(88, 120), scalar=-1e30, in1=V(56, 88),
                                                          op0=ALU.mult, op1=ALU.add))
                dv(lambda: nc.vector.tensor_reduce(out=V(55, 56), in_=V(56, 88), axis=AX.X, op=ALU.max))
                dv(lambda: nc.vector.tensor_scalar(out=V(120, 152), in0=V(56, 88), scalar1=V(55, 56), scalar2=None, op0=ALU.is_equal))
                dv(lambda: nc.vector.tensor_tensor(out=V(152, 153), in0=V(55, 56), in1=V(54, 55), op=ALU.subtract))
                S.op("act", lambda: nc.scalar.activation(out=V(153, 154), in_=V(152, 153), func=AF.Exp), reads=[tb], writes=[tb])
                dv(lambda: nc.vector.tensor_scalar(out=V(154, 155), in0=V(153, 154), scalar1=1.0, scalar2=None, op0=ALU.add))
                dv(lambda: nc.vector.reciprocal(out=V(154, 155), in_=V(154, 155)))
                dv(lambda: nc.vector.tensor_tensor(out=V(155, 156), in0=V(153, 154), in1=V(154, 155), op=ALU.mult))
                dv(lambda: nc.vector.tensor_scalar(out=V(156, 158), in0=V(154, 156), scalar1=V(49, 50), scalar2=None, op0=ALU.mult))
                S.op("dve", lambda: nc.vector.tensor_tensor(out=oh_all[:, tt, :], in0=V(88, 120), in1=V(120, 152), op=ALU.add),
                     reads=[tb], writes=[OHB[tt]])
                for j in range(tt + 1):
                    S.op("pe", lambda j=j: nc.tensor.matmul(ps[5][:, 0:NE], lhsT=(stri_b[:] if j == tt else ones_b[:]),
                                                            rhs=oh_all[:, j, :], start=(j == 0), stop=(j == tt)),
                         reads=[OHB[j], CB], writes=[pb[5]], inc=(j == tt))
                dv(lambda: nc.vector.tensor_copy(out=V(56, 88), in_=ps[5][:, 0:NE]), rd=[pb[5]])
                dv(lambda: nc.vector.tensor_tensor(out=V(4, 36), in0=V(56, 88), in1=ebase[:], op=ALU.add), rd=[RP])
                for k, (o0, o1) in enumerate(((88, 120), (120, 152))):
                    dv(lambda o0=o0, o1=o1: nc.vector.tensor_tensor(out=V(o0, o1), in0=V(o0, o1), in1=V(4, 36), op=ALU.mult))
                    dv(lambda o0=o0, o1=o1: nc.vector.tensor_reduce(out=V(0, 1), in_=V(o0, o1), axis=AX.X, op=ALU.add))
                    dv(lambda o0=o0, o1=o1: nc.vector.tensor_tensor(out=V(o0, o1), in0=V(o0, o1), in1=ebase[:], op=ALU.subtract), rd=[RP])
                    dv(lambda o0=o0, o1=o1: nc.vector.tensor_reduce(out=V(2, 3), in_=V(o0, o1), axis=AX.X, op=ALU.max))
                    dv(lambda: nc.vector.tensor_scalar(out=V(2, 3), in0=V(2, 3), scalar1=float(CE), scalar2=None, op0=ALU.is_ge))
                    dv(lambda: nc.vector.scalar_tensor_tensor(out=V(0, 1), in0=V(2, 3), scalar=1e6, in1=V(0, 1), op0=ALU.mult, op1=ALU.add))
                    dv(lambda: nc.vector.tensor_scalar(out=V(2, 3), in0=V(2, 3), scalar1=-1.0, scalar2=1.0, op0=ALU.mult, op1=ALU.add))
                    dv(lambda k=k: nc.vector.tensor_tensor(out=gate_all[:, tt, k:k + 1], in0=V(156 + k, 157 + k), in1=V(2, 3), op=ALU.mult), wr_=[GD])
                    dv(lambda k=k: nc.vector.tensor_copy(out=dest_all[:, tt, k:k + 1], in_=V(0, 1)), wr_=[GD])
                    S.dma("pool", lambda k=k: nc.gpsimd.indirect_dma_start(
                        out=xs_d[:, :], out_offset=bass.IndirectOffsetOnAxis(ap=dest_all[:, tt, k:k + 1], axis=0),
                        in_=h_t[:, :], in_offset=None, bounds_check=NE * CE - 1, oob_is_err=False), h_ch, reads=[h_b, GD])
            S.barrier()

    def experts():
        elist = dbg.get("experts", list(range(NE)))
        with ExitStack() as st:
            xgr = Rot(S, st, "xg", [128, D], BF16, 2, dma=True)
            xTr = Rot(S, st, "xgT", [128, KC, 128], BF16, 2)
            wgu = Rot(S, st, "wgu", [128, KC, 512], BF16, 2, dma=True)
            wdr = Rot(S, st, "wdn", [128, 8, 1024], BF16, 2, dma=True)
            gsr = Rot(S, st, "gsb", [128, 512], F32, 2)
            acr = Rot(S, st, "actv", [128, FF], BF16, 2)
            aTr = Rot(S, st, "actT", [128, 8, 128], BF16, 2)
            ysr = Rot(S, st, "yst", [128, 512], F32, 3, dma=True)
            for wi, e in enumerate(elist):
                x_t, x_b, x_ch = xgr.next()
                S.dma("sp", lambda x_t=x_t, e=e: nc.sync.dma_start(out=x_t[:], in_=xs_d[e * CE:(e + 1) * CE, :]), x_ch, writes=[x_b])
                xT_t, xT_b = xTr.next()
                transpose_rows(x_t, x_b, xT_t, xT_b, 0, evac_alt=wi)
                a_t, a_b = acr.next()
                wgv = w_gate[wi].rearrange("(kc p) f -> p kc f", p=128)
                wuv = w_up[wi].rearrange("(kc p) f -> p kc f", p=128)
                for half in range(2):
                    hs = slice(half * 512, (half + 1) * 512)
                    g_w, g_wb, g_ch = wgu.next()
                    cbf = conv_bufs.get((wi, "g", half))
                    if cbf is not None:
                        gsrc = wbg_d[wi].rearrange("(kc p) f -> p kc f", p=128)[:, :, hs]
                        S.dma("pool", lambda g_w=g_w, gsrc=gsrc: nc.gpsimd.dma_start(out=g_w[:], in_=gsrc), g_ch, reads=[cbf], writes=[g_wb])
                    else:
                        S.dma("pool", lambda g_w=g_w, hs=hs, wgv=wgv: nc.gpsimd.dma_start(out=g_w[:], in_=wgv[:, :, hs]), g_ch, writes=[g_wb])
                    for kc in range(KC):
                        S.op("pe", lambda kc=kc, g_w=g_w: nc.tensor.matmul(ps[4][:], lhsT=xT_t[:, kc, :], rhs=g_w[:, kc, :],
                                                                          start=(kc == 0), stop=(kc == KC - 1)),
                             reads=[xT_b, g_wb], writes=[pb[4]], inc=(kc == KC - 1))
                    u_w, u_wb, u_ch = wgu.next()
                    cbf = conv_bufs.get((wi, "u", half))
                    if cbf is not None:
                        usrc = wbu_d[wi].rearrange("(kc p) f -> p kc f", p=128)[:, :, hs]
                        S.dma("pool", lambda u_w=u_w, usrc=usrc: nc.gpsimd.dma_start(out=u_w[:], in_=usrc), u_ch, reads=[cbf], writes=[u_wb])
                    else:
                        S.dma("pool", lambda u_w=u_w, hs=hs, wuv=wuv: nc.gpsimd.dma_start(out=u_w[:], in_=wuv[:, :, hs]), u_ch, writes=[u_wb])
                    for kc in range(KC):
                        S.op("pe", lambda kc=kc, u_w=u_w: nc.tensor.matmul(ps[5][:], lhsT=xT_t[:, kc, :], rhs=u_w[:, kc, :],
                                                                          start=(kc == 0), stop=(kc == KC - 1)),
                             reads=[xT_b, u_wb], writes=[pb[5]], inc=(kc == KC - 1))
                    gs_t, gs_b = gsr.next()
                    S.op("act", lambda gs_t=gs_t: nc.scalar.activation(out=gs_t[:], in_=ps[4][:], func=AF.Silu), reads=[pb[4]], writes=[gs_b])
                    S.op("dve", lambda gs_t=gs_t, hs=hs: nc.vector.tensor_tensor(out=a_t[:, hs], in0=gs_t[:], in1=ps[5][:], op=ALU.mult),
                         reads=[gs_b, pb[5]], writes=[a_b])
                aT_t, aT_b = aTr.next()
                pbf = ps[6][:].bitcast(BF16)
                for j in range(8):
                    S.op("pe", lambda j=j: nc.tensor.transpose(out=pbf[:, j * 128:(j + 1) * 128], in_=a_t[:, j * 128:(j + 1) * 128],
                                                               identity=ident_b[:]), reads=[a_b, CB], writes=[pb[6]], inc=(j == 7))
                S.op("act", lambda: nc.scalar.copy(out=aT_t[:], in_=pbf.rearrange("p (j t) -> p j t", j=8)), reads=[pb[6]], writes=[aT_b])
                wdv = w_down[wi].rearrange("(fk p) d -> p fk d", p=128)
                for q in range(4):
                    d_w, d_wb, d_ch = wdr.next()
                    cbf = conv_bufs.get((wi, "d", q // 2))
                    if cbf is not None:
                        dsrc = wbd_d[wi].rearrange("(fk p) d -> p fk d", p=128)[:, :, q * 1024:(q + 1) * 1024]
                        S.dma("pool", lambda d_w=d_w, dsrc=dsrc: nc.gpsimd.dma_start(out=d_w[:], in_=dsrc), d_ch, reads=[cbf], writes=[d_wb])
                    else:
                        S.dma("pool", lambda d_w=d_w, q=q, wdv=wdv: nc.gpsimd.dma_start(out=d_w[:], in_=wdv[:, :, q * 1024:(q + 1) * 1024]),
                              d_ch, writes=[d_wb])
                    for cbk in range(2):
                        bank = (q * 2 + cbk) % 4
                        for fk in range(8):
                            S.op("pe", lambda fk=fk, bank=bank, cbk=cbk, d_w=d_w: nc.tensor.matmul(
                                ps[bank][:], lhsT=aT_t[:, fk, :], rhs=d_w[:, fk, cbk * 512:(cbk + 1) * 512],
                                start=(fk == 0), stop=(fk == 7)), reads=[aT_b, d_wb], writes=[pb[bank]], inc=(fk == 7))
                        y_t, y_b, y_ch = ysr.next()
                        if cbk == 0:
                            S.op("act", lambda y_t=y_t, bank=bank: nc.scalar.copy(out=y_t[:], in_=ps[bank][:]), reads=[pb[bank]], writes=[y_b])
                        else:
                            S.op("dve", lambda y_t=y_t, bank=bank: nc.vector.tensor_copy(out=y_t[:], in_=ps[bank][:]), reads=[pb[bank]], writes=[y_b])
                        c0 = q * 1024 + cbk * 512
                        S.dma("sp", lambda y_t=y_t, e=e, c0=c0: nc.sync.dma_start(out=ys_d[e * CE:(e + 1) * CE, c0:c0 + 512], in_=y_t[:]),
                              y_ch, reads=[y_b])
            S.barrier()

    def combine():
        with ExitStack() as st:
            y1r = Rot(S, st, "y1", [128, D], F32, 2, dma=True)
            y2r = Rot(S, st, "y2", [128, D], F32, 2, dma=True)
            xr = Rot(S, st, "x2o", [128, D], F32, 2, dma=True)
            for r in (y1r, y2r):
                for i in range(2):
                    S.op("dve", lambda r=r, i=i: nc.vector.memset(r.t[i][:], 0.0), writes=[r.b[i]])
            for tt in range(NT):
                x_t, x_b, x_ch = xr.next()
                S.dma("sp", lambda x_t=x_t, tt=tt: nc.sync.dma_start(out=x_t[:], in_=x2_d[tt * 128:(tt + 1) * 128, :]), x_ch, writes=[x_b])
                ys = []
                for k, rr in enumerate((y1r, y2r)):
                    y_t, y_b, y_ch = rr.next()
                    S.dma("pool", lambda y_t=y_t, k=k, tt=tt: nc.gpsimd.indirect_dma_start(
                        out=y_t[:, :], out_offset=None, in_=ys_d[:, :],
                        in_offset=bass.IndirectOffsetOnAxis(ap=dest_all[:, tt, k:k + 1], axis=0),
                        bounds_check=NE * CE - 1, oob_is_err=False), y_ch, reads=[GD], writes=[y_b])
                    ys.append((y_t, y_b))
                for k, (y_t, y_b) in enumerate(ys):
                    S.op("dve", lambda y_t=y_t, k=k, x_t=x_t, tt=tt: nc.vector.scalar_tensor_tensor(
                        out=x_t[:], in0=y_t[:], scalar=gate_all[:, tt, k:k + 1], in1=x_t[:], op0=ALU.mult, op1=ALU.add),
                        reads=[y_b, GD, x_b], writes=[x_b])
                S.dma("sp", lambda x_t=x_t, tt=tt: nc.sync.dma_start(out=out_d[tt * 128:(tt + 1) * 128, :], in_=x_t[:]), x_ch, reads=[x_b])
            S.barrier()

    if stop not in ("A", "Bpre", "B", "D"):
        if not dbg.get("skip_wout"):
            wout()
        route()
        if stop != "E":
            experts()
            combine()

    S.barrier(final=True)
    es.close()
    return nc, S


def _consts():
    p = np.arange(128)
    c = {}
    c["c_ident"] = np.eye(128, dtype=np.float32)
    sw = np.zeros((128, 128), np.float32)
    sw[(p + 64) % 128, p] = 1.0
    c["c_pswap"] = sw
    c["c_tri"] = (p[:, None] <= p[None, :]).astype(np.float32)
    c["c_stri"] = (p[:, None] < p[None, :]).astype(np.float32)
    c["c_negm"] = np.where(p[None, :] < p[:, None], -30000.0, 0.0).astype(np.float32)
    kj = p[:, None]
    am = np.zeros((128, 16, 128), np.float32)
    for Dd in range(16):
        d = Dd * 128 + p[None, :] - kj
        m = ((d >= 0) & (d <= 128)).astype(np.float32)
        m += ((d >= 0) & (d % 4 == 0) & (d <= 512))
        m += ((d >= 0) & (d % 16 == 0) & (d <= 2048))
        am[:, Dd, :] = m
    c["c_amask"] = am.reshape(128, 2048)
    inv = np.power(np.float32(10000.0), -np.arange(64, dtype=np.float32) / 64).astype(np.float32)
    rope = np.zeros((128, 2), np.float32)
    rope[:, 0] = np.tile(inv, 2) / np.float32(2 * np.pi)
    rope[:64, 1] = -TWO_PI
    rope[64:, 1] = TWO_PI
    c["c_rope"] = rope
    c["c_ebase"] = np.tile((np.arange(NE) * CE).astype(np.float32)[None, :], (128, 1))
    return c


def make_in_maps(inp):
    f = lambda a: np.ascontiguousarray(np.asarray(a, dtype=np.float32))
    x = f(inp["x"])
    pos = np.ascontiguousarray(np.asarray(inp["positions"], dtype=np.int32))
    shared = {
        "w_in": f(inp["w_in"][0]), "w_out": f(inp["w_out"][0]),
        "w_gate": f(inp["w_gate"][0]), "w_up": f(inp["w_up"][0]), "w_down": f(inp["w_down"][0]),
        "norm_attn_w": f(inp["norm_attn_w"][0]), "norm_ffn_w": f(inp["norm_ffn_w"][0]),
        "nfw_col": f(np.asarray(inp["norm_ffn_w"][0]).reshape(KC, 128).T),
        "qk_w": f(np.stack([np.asarray(inp["q_norm_w"][0]), np.asarray(inp["k_norm_w"][0])], axis=1)),
        "conv_wc": f(np.asarray(inp["conv_w"][0]).reshape(4, 32, 128).transpose(2, 1, 0)),
        "conv_bc": f(np.asarray(inp["conv_b"][0]).reshape(32, 128).T),
        "dt_bias": f(inp["dt_bias"][0]), "a_log": f(inp["a_log"][0]),
        "d_rep": f(np.repeat(np.asarray(inp["d_skip"][0]), 64)),
        "ssd_nw": f(inp["ssd_norm_w"][0]),
        "w_r": f(np.concatenate([np.asarray(inp["router_group_w"][0]), np.asarray(inp["router_expert_w"][0])],
                                axis=1).reshape(KC, 128, 36).transpose(1, 0, 2)),
        "b_r": f(np.concatenate([np.asarray(inp["router_group_b"][0]), np.asarray(inp["router_expert_b"][0])])),
    }
    shared.update(_consts())
    maps = []
    for c in range(8):
        b, th = c // 2, c % 2
        m = dict(shared)
        m["x_own"] = np.ascontiguousarray(x[b, th * NTOK:(th + 1) * NTOK])
        m["pos_own"] = np.ascontiguousarray(pos[b, th * NTOK:(th + 1) * NTOK])
        if th == 1:
            m["x_pre"] = np.ascontiguousarray(x[b, 0:NTOK])
            m["pos_pre"] = np.ascontiguousarray(pos[b, 0:NTOK])
            m["vflag"] = np.ones((128, NT), np.float32)
        else:
            m["x_pre"] = np.zeros((NTOK, D), np.float32)
            m["pos_pre"] = np.zeros((NTOK,), np.int32)
            m["vflag"] = np.zeros((128, NT), np.float32)
        maps.append(m)
    return maps


_PROG = {}


def kernel(**inputs):
    if "nc" not in _PROG:
        _PROG["nc"] = build_program()[0]
    maps = make_in_maps(inputs)
    res = run_bass_kernel_spmd(_PROG["nc"], maps, core_ids=list(range(8)))
    out = np.zeros((4, 2 * NTOK, D), np.float32)
    for c in range(8):
        b, th = c // 2, c % 2
        out[b, th * NTOK:(th + 1) * NTOK] = res.results[c]["out"]
    return out
```

```python
import numpy as np
from contextlib import ExitStack
import concourse.bass as bass
import concourse.mybir as mybir
from concourse.bass_utils import run_bass_kernel_spmd

F32 = mybir.dt.float32
BF16 = mybir.dt.bfloat16
I32 = mybir.dt.int32
AF = mybir.ActivationFunctionType
ALU = mybir.AluOpType
AX = mybir.AxisListType

D = 4096
NTOK = 1024
NT = NTOK // 128
KC = D // 128
NH = 16
NE = 32
CE = 128
FF = 1024
TWO_PI = 6.283179


class Buf:
    __slots__ = ("name", "lw", "rd", "psum")

    def __init__(self, name, psum=False):
        self.name = name
        self.lw = {}
        self.rd = {}
        self.psum = psum


class Sched:
    ENG = ("pe", "act", "dve", "pool", "sp")

    def __init__(self, nc, es):
        self.nc = nc
        self.es = es
        self.e = {"pe": nc.tensor, "act": nc.scalar, "dve": nc.vector, "pool": nc.gpsimd, "sp": nc.sync}
        self.csem = {k: es.enter_context(nc.semaphore("c_" + k)) for k in ("pe", "act", "dve", "pool")}
        self.ccnt = {k: 0 for k in self.csem}
        self.dsems = []
        self.known = {k: {} for k in self.ENG}
        self.nwaits = 0
        self.bg_chans = set()

    def chan(self):
        s = self.es.enter_context(self.nc.semaphore("d%d" % len(self.dsems)))
        self.dsems.append([s, 0])
        return len(self.dsems) - 1

    def _wait(self, E, kk, val):
        kind, key = kk
        if kind == "c":
            if key == "pe" and E == "pe":
                return
            if val > self.ccnt[key]:
                raise RuntimeError("wait on an event that is not issued yet: %s %d" % (key, val))
            sem = self.csem[key]
        else:
            sem, val = self.dsems[key]
        if self.known[E].get(kk, 0) >= val:
            return
        self.e[E].wait_ge(sem, val)
        self.nwaits += 1
        self.known[E][kk] = val

    def _deps(self, E, reads, writes):
        need = {}
        for b in reads:
            for kk, v in b.lw.items():
                need[kk] = max(need.get(kk, 0), v)
        for b in writes:
            for kk, v in b.lw.items():
                need[kk] = max(need.get(kk, 0), v)
            for kk, v in b.rd.items():
                need[kk] = max(need.get(kk, 0), v)
        for kk, v in need.items():
            self._wait(E, kk, v)

    def _record(self, kk, val, reads, writes):
        for b in reads:
            b.rd[kk] = max(b.rd.get(kk, 0), val)
        for b in writes:
            b.lw = {kk: val}
            b.rd = {}

    def op(self, E, fn, reads=(), writes=(), inc=True):
        if E != "pe":
            ex = [b for b in reads if b.psum]
            if ex:
                writes = list(writes) + ex
        self._deps(E, reads, writes)
        ins = fn()
        if inc:
            self.ccnt[E] += 1
            ins.then_inc(self.csem[E], 1)
            val = self.ccnt[E]
        else:
            assert E == "pe"
            val = self.ccnt[E] + 1
        self._record(("c", E), val, reads, writes)
        return ins

    def dma(self, Q, fn, ch, reads=(), writes=()):
        self._deps(Q, reads, writes)
        ins = fn()
        self.dsems[ch][1] += 16
        ins.then_inc(self.dsems[ch][0], 16)
        self._record(("d", ch), self.dsems[ch][1], reads, writes)
        return ins

    def barrier(self, final=False):
        for E in self.ENG:
            for k in self.csem:
                self._wait(E, ("c", k), self.ccnt[k])
            for i in range(len(self.dsems)):
                if i in self.bg_chans and not final:
                    continue
                self._wait(E, ("d", i), 0)


_UID = [0]


class Rot:
    def __init__(self, S, st, name, shape, dtype, n, dma=False):
        _UID[0] += 1
        self.t = [st.enter_context(S.nc.sbuf_tensor("%s_%d_%d" % (name, _UID[0], i), shape, dtype)) for i in range(n)]
        self.b = [Buf("%s%d" % (name, i)) for i in range(n)]
        self.ch = [S.chan() for _ in range(n)] if dma else None
        self.i = -1
        self.n = n

    def next(self):
        self.i = (self.i + 1) % self.n
        if self.ch is None:
            return self.t[self.i], self.b[self.i]
        return self.t[self.i], self.b[self.i], self.ch[self.i]


def build_program(dbg=None):
    dbg = dbg or {}
    stop = dbg.get("stop")
    exported = dbg.get("export", ())
    nc = bass.Bass("TRN2", target_bir_lowering=False)
    es = ExitStack()
    S = Sched(nc, es)

    def din(name, shape, dt=F32):
        if name in dbg.get("dummy", ()):
            shape = [1, 1]
        return nc.dram_tensor(name, list(shape), dt, kind="ExternalInput").ap()

    inject = dbg.get("inject", ())

    def dscr(name, shape, dt):
        kind = "ExternalOutput" if name in exported else ("ExternalInput" if name in inject else "Internal")
        return nc.dram_tensor(name, list(shape), dt, kind=kind).ap()

    x_own = din("x_own", [NTOK, D])
    x_pre = din("x_pre", [NTOK, D])
    pos_own = din("pos_own", [NTOK], I32)
    pos_pre = din("pos_pre", [NTOK], I32)
    vflag_d = din("vflag", [128, NT])
    w_in = din("w_in", [D, 12320])
    w_out = din("w_out", [D, D])
    need_moe = stop in (None, "G")
    if need_moe:
        nexp = len(dbg.get("experts", range(NE)))
        w_gate = din("w_gate", [nexp, D, FF])
        w_up = din("w_up", [nexp, D, FF])
        w_down = din("w_down", [nexp, FF, D])
    norm_attn_w = din("norm_attn_w", [D])
    norm_ffn_w = din("norm_ffn_w", [D])
    nfw_col = din("nfw_col", [128, KC])
    qk_w = din("qk_w", [128, 2])
    conv_wc = din("conv_wc", [128, 32, 4])
    conv_bc = din("conv_bc", [128, 32])
    dt_bias = din("dt_bias", [32])
    a_log = din("a_log", [32])
    d_rep = din("d_rep", [2048])
    ssd_nw = din("ssd_nw", [2048])
    w_r = din("w_r", [128, KC, 36])
    b_r = din("b_r", [36])
    c_ident = din("c_ident", [128, 128])
    c_pswap = din("c_pswap", [128, 128])
    c_tri = din("c_tri", [128, 128])
    c_stri = din("c_stri", [128, 128])
    c_negm = din("c_negm", [128, 128])
    c_amask = din("c_amask", [128, 16 * 128])
    c_rope = din("c_rope", [128, 2])
    c_ebase = din("c_ebase", [128, NE])
    out_d = nc.dram_tensor("out", [NTOK, D], F32, kind="ExternalOutput").ap()

    qT_d = dscr("s_qT", [NH, 128, NTOK], BF16)
    kT_d = dscr("s_kT", [NH, 128, 2 * NTOK], BF16)
    v_d = dscr("s_v", [2 * NTOK, 2048], BF16)
    zs_d = dscr("s_zs", [NTOK, 2048], BF16)
    xbcT_d = dscr("s_xbcT", [4096, 2 * NTOK], F32)
    dtr_d = dscr("s_dtr", [2 * NTOK, 32], F32)
    mix_d = dscr("s_mix", [NTOK, D], BF16)
    x2_d = dscr("s_x2", [NTOK, D], F32)
    xs_d = dscr("s_xs", [NE * CE, D], BF16)
    ys_d = dscr("s_ys", [NE * CE, D], F32)
    dbg_d = dscr("s_dbg", [NTOK, 64], F32)

    def sb(st, name, shape, dt):
        _UID[0] += 1
        return st.enter_context(nc.sbuf_tensor("%s_%d" % (name, _UID[0]), list(shape), dt))

    ident_f = sb(es, "ident_f", [128, 128], F32)
    ident_b = sb(es, "ident_b", [128, 128], BF16)
    ones_b = sb(es, "ones_b", [128, 128], BF16)
    ones_f = sb(es, "ones_f", [128, 128], F32)
    pswap_b = sb(es, "pswap_b", [128, 128], BF16)
    tri_f = sb(es, "tri_f", [128, 128], F32)
    stri_b = sb(es, "stri_b", [128, 128], BF16)
    negm_f = sb(es, "negm_f", [128, 128], F32)
    amask = sb(es, "amask", [128, 16 * 128], BF16)
    rope_c = sb(es, "rope_c", [128, 2], F32)
    qkw = sb(es, "qkw", [128, 2], F32)
    vflag = sb(es, "vflag_sb", [128, NT], F32)
    eps6 = sb(es, "eps6", [128, 1], F32)
    eps5 = sb(es, "eps5", [128, 1], F32)
    zero_b = sb(es, "zero_b", [128, D], BF16)
    CB = Buf("consts")
    cch = S.chan()
    for t, src in ((ident_f, c_ident), (tri_f, c_tri), (negm_f, c_negm), (rope_c, c_rope), (qkw, qk_w),
                   (vflag, vflag_d)):
        S.dma("sp", lambda t=t, src=src: nc.sync.dma_start(out=t[:], in_=src), cch, writes=[CB])
    for t, src in ((ident_b, c_ident), (pswap_b, c_pswap), (stri_b, c_stri), (amask, c_amask)):
        S.dma("pool", lambda t=t, src=src: nc.gpsimd.dma_start(out=t[:], in_=src), cch, writes=[CB])
    S.op("dve", lambda: nc.vector.memset(ones_b[:], 1.0), writes=[CB])
    S.op("dve", lambda: nc.vector.memset(ones_f[:], 1.0), writes=[CB])
    S.op("dve", lambda: nc.vector.memset(eps6[:], 1e-6), writes=[CB])
    S.op("dve", lambda: nc.vector.memset(eps5[:], 1e-5), writes=[CB])
    S.op("dve", lambda: nc.vector.memset(zero_b[:], 0.0), writes=[CB])
    S.op("dve", lambda: nc.vector.tensor_scalar(out=qkw[:, 0:1], in0=qkw[:, 0:1], scalar1=float(128 ** -0.5),
                                                scalar2=None, op0=ALU.mult), reads=[CB], writes=[CB])
    zch = S.chan()
    for e in range(NE):
        S.dma("sp", lambda e=e: nc.sync.dma_start(out=xs_d[e * CE:(e + 1) * CE, :], in_=zero_b[:]), zch, reads=[CB])

    ps = [es.enter_context(nc.psum_tensor("ps%d" % i, [128, 512], F32)) for i in range(8)]
    pb = [Buf("ps%d" % i, psum=True) for i in range(8)]
    S.barrier()

    def proj_pass(x_d, pos_d, own):
        tok0 = NTOK if own else 0
        with ExitStack() as st:
            hT = sb(st, "hT", [128, KC, NTOK], BF16)
            hTb = [Buf("hT%d" % i) for i in range(NT)]
            cosT = sb(st, "cosT", [128, NTOK], F32)
            sinT = sb(st, "sinT", [128, NTOK], F32)
            TB = Buf("rope_tab")
            with ExitStack() as sa:
                wbc = sb(sa, "wbc", [128, D], F32)
                WB = Buf("wbc")
                S.dma("sp", lambda: nc.sync.dma_start(out=wbc[:], in_=norm_attn_w.partition_broadcast(128)),
                      cch, writes=[WB])
                posi = sb(sa, "posi", [128, NTOK], I32)
                yv = sb(sa, "yv", [128, NTOK], F32)
                yi = sb(sa, "yi", [128, NTOK], I32)
                yf = sb(sa, "yf", [128, NTOK], F32)
                S.dma("sp", lambda: nc.sync.dma_start(out=posi[:], in_=pos_d.partition_broadcast(128)), cch,
                      writes=[TB])
                S.op("dve", lambda: nc.vector.tensor_copy(out=yf[:], in_=posi[:]), reads=[TB], writes=[TB])
                S.op("dve", lambda: nc.vector.tensor_scalar(out=yv[:], in0=yf[:], scalar1=rope_c[:, 0:1],
                                                            scalar2=None, op0=ALU.mult), reads=[TB, CB], writes=[TB])
                for tab, shift in ((sinT, 0.0), (cosT, 0.25)):
                    if shift:
                        S.op("dve", lambda: nc.vector.tensor_scalar(out=yv[:], in0=yv[:], scalar1=shift,
                                                                    scalar2=None, op0=ALU.add), reads=[TB], writes=[TB])
                    S.op("dve", lambda: nc.vector.tensor_copy(out=yi[:], in_=yv[:]), reads=[TB], writes=[TB])
                    S.op("dve", lambda: nc.vector.tensor_copy(out=yf[:], in_=yi[:]), reads=[TB], writes=[TB])
                    S.op("dve", lambda: nc.vector.tensor_tensor(out=yf[:], in0=yv[:], in1=yf[:], op=ALU.subtract),
                         reads=[TB], writes=[TB])
                    if shift:
                        S.op("act", lambda tab=tab: nc.scalar.activation(out=tab[:], in_=yf[:], func=AF.Sin,
                                                                         scale=TWO_PI), reads=[TB], writes=[TB])
                    else:
                        S.op("act", lambda tab=tab: nc.scalar.activation(out=tab[:], in_=yf[:], func=AF.Sin,
                                                                         scale=rope_c[:, 1:2]), reads=[TB, CB], writes=[TB])
                xt = Rot(S, sa, "xt", [128, D], F32, 2, dma=True)
                hb = Rot(S, sa, "hb", [128, D], BF16, 2)
                junk = sb(sa, "junk", [128, D], BF16)
                JB = Buf("junk")
                ssr = Rot(S, sa, "ss", [128, 2], F32, 2)
                for tt in range(NT):
                    x_t, x_b, x_ch = xt.next()
                    S.dma("sp", lambda x_t=x_t, tt=tt: nc.sync.dma_start(out=x_t[:], in_=x_d[tt * 128:(tt + 1) * 128, :]),
                          x_ch, writes=[x_b])
                    s_t, s_b = ssr.next()
                    S.op("act", lambda x_t=x_t, s_t=s_t: nc.scalar.activation(out=junk[:], in_=x_t[:], func=AF.Square,
                                                                              accum_out=s_t[:, 0:1]),
                         reads=[x_b], writes=[JB, s_b])
                    S.op("act", lambda s_t=s_t: nc.scalar.activation(out=s_t[:, 1:2], in_=s_t[:, 0:1], func=AF.Sqrt,
                                                                     scale=1.0 / D, bias=eps6[:, 0:1]),
                         reads=[s_b, CB], writes=[s_b])
                    S.op("dve", lambda s_t=s_t: nc.vector.reciprocal(out=s_t[:, 1:2], in_=s_t[:, 1:2]), reads=[s_b], writes=[s_b])
                    h_t, h_b = hb.next()
                    S.op("dve", lambda h_t=h_t, x_t=x_t, s_t=s_t: nc.vector.scalar_tensor_tensor(
                        out=h_t[:], in0=x_t[:], scalar=s_t[:, 1:2], in1=wbc[:], op0=ALU.mult, op1=ALU.mult),
                        reads=[x_b, s_b, WB], writes=[h_b])
                    for q4 in range(4):
                        bank = q4 % 4
                        pbf = ps[bank][:].bitcast(BF16)
                        for j in range(8):
                            kc = q4 * 8 + j
                            S.op("pe", lambda pbf=pbf, j=j, kc=kc, h_t=h_t: nc.tensor.transpose(
                                out=pbf[:, j * 128:(j + 1) * 128], in_=h_t[:, kc * 128:(kc + 1) * 128], identity=ident_b[:]),
                                reads=[h_b, CB], writes=[pb[bank]], inc=(j == 7))
                        src = pbf.rearrange("p (j t) -> p j t", j=8)
                        dst = hT[:, q4 * 8:(q4 + 1) * 8, tt * 128:(tt + 1) * 128]
                        if q4 % 2 == 0:
                            S.op("act", lambda dst=dst, src=src: nc.scalar.copy(out=dst, in_=src), reads=[pb[bank]], writes=[hTb[tt]])
                        else:
                            S.op("dve", lambda dst=dst, src=src: nc.vector.tensor_copy(out=dst, in_=src), reads=[pb[bank]], writes=[hTb[tt]])
                S.barrier()
            if stop == "A":
                return
            wv = w_in.rearrange("(kc p) n -> p kc n", p=128)
            wt = Rot(S, st, "wt", [128, KC, 512], BF16, 2, dma=True)
            ev32 = Rot(S, st, "ev32", [128, 512], F32, 3, dma=True)
            evb = Rot(S, st, "evb", [128, 512], BF16, 3, dma=True)
            sqr = Rot(S, st, "sq", [128, 512], BF16, 2)
            rsr = Rot(S, st, "rs", [128, 512], F32, 2)
            qnr = Rot(S, st, "qn", [128, 512], BF16, 2)
            t1r = Rot(S, st, "t1", [128, 512], F32, 2)
            t2r = Rot(S, st, "t2", [128, 512], F32, 2)
            items = []
            if own:
                items += [("q", c0) for c0 in range(0, 2048, 512)]
            items += [("k", c0) for c0 in range(2048, 4096, 512)]
            items += [("v", c0) for c0 in range(4096, 6144, 512)]
            if own:
                items += [("z", c0) for c0 in range(6144, 8192, 512)]
            items += [("f", c0) for c0 in range(8192, 12288, 512)]
            items += [("dt", 12288)]
            if dbg.get("items"):
                items = [it for it in items if it[0] in dbg["items"]]
            mainbank = [0]
            auxbank = [0, 0]
            pending = []

            def step_pending(flush=False):
                while True:
                    for g in list(pending):
                        try:
                            next(g)
                        except StopIteration:
                            pending.remove(g)
                    if not flush or not pending:
                        break

            def qk_post(kind, head, tb, bank):
                wcol = qkw[:, 0:1] if kind == "q" else qkw[:, 1:2]
                sq_t, sq_b = sqr.next()
                S.op("act", lambda: nc.scalar.activation(out=sq_t[:], in_=ps[bank][:], func=AF.Square),
                     reads=[pb[bank]], writes=[sq_b])
                yield
                b2 = 4 + auxbank[0] % 2
                auxbank[0] += 1
                S.op("pe", lambda: nc.tensor.matmul(ps[b2][:], lhsT=ones_b[:], rhs=sq_t[:], start=True, stop=True),
                     reads=[sq_b, CB], writes=[pb[b2]])
                rs_t, rs_b = rsr.next()
                S.op("act", lambda: nc.scalar.activation(out=rs_t[:], in_=ps[b2][:], func=AF.Sqrt, scale=1.0 / 128,
                                                         bias=eps6[:, 0:1]), reads=[pb[b2], CB], writes=[rs_b])
                S.op("dve", lambda: nc.vector.reciprocal(out=rs_t[:], in_=rs_t[:]), reads=[rs_b], writes=[rs_b])
                qn_t, qn_b = qnr.next()
                S.op("dve", lambda: nc.vector.scalar_tensor_tensor(out=qn_t[:], in0=ps[bank][:], scalar=wcol, in1=rs_t[:],
                                                                   op0=ALU.mult, op1=ALU.mult),
                     reads=[pb[bank], rs_b, CB], writes=[qn_b])
                yield
                b3 = 6 + auxbank[1] % 2
                auxbank[1] += 1
                S.op("pe", lambda: nc.tensor.matmul(ps[b3][:], lhsT=pswap_b[:], rhs=qn_t[:], start=True, stop=True),
                     reads=[qn_b, CB], writes=[pb[b3]])
                t1_t, t1_b = t1r.next()
                t2_t, t2_b = t2r.next()
                cs = slice(tb * 512, (tb + 1) * 512)
                S.op("dve", lambda: nc.vector.tensor_tensor(out=t1_t[:], in0=qn_t[:], in1=cosT[:, cs], op=ALU.mult),
                     reads=[qn_b, TB], writes=[t1_b])
                S.op("dve", lambda: nc.vector.tensor_tensor(out=t2_t[:], in0=ps[b3][:], in1=sinT[:, cs], op=ALU.mult),
                     reads=[pb[b3], TB], writes=[t2_b])
                o_t, o_b, o_ch = evb.next()
                S.op("dve", lambda: nc.vector.tensor_tensor(out=o_t[:], in0=t1_t[:], in1=t2_t[:], op=ALU.add),
                     reads=[t1_b, t2_b], writes=[o_b])
                if kind == "q":
                    dst = qT_d[head, :, tb * 512:(tb + 1) * 512]
                else:
                    dst = kT_d[head, :, tok0 + tb * 512: tok0 + (tb + 1) * 512]
                S.dma("sp", lambda: nc.sync.dma_start(out=dst, in_=o_t[:]), o_ch, reads=[o_b])

            for kind, c0 in items:
                ncol = 32 if kind == "dt" else 512
                w_t, w_b, w_ch = wt.next()
                S.dma("pool", lambda w_t=w_t, c0=c0, ncol=ncol: nc.gpsimd.dma_start(
                    out=w_t[:, :, 0:ncol], in_=wv[:, :, c0:c0 + ncol]), w_ch, writes=[w_b])
                if NCV_CHUNKS and kind != "dt":
                    bg_convert(1)
                if kind in ("q", "k", "f"):
                    for sub in range(4):
                        for tb in range(2):
                            bank = mainbank[0] % 4
                            mainbank[0] += 1
                            for kc in range(KC):
                                S.op("pe", lambda bank=bank, kc=kc, sub=sub, tb=tb, w_t=w_t: nc.tensor.matmul(
                                    ps[bank][:], lhsT=w_t[:, kc, sub * 128:(sub + 1) * 128],
                                    rhs=hT[:, kc, tb * 512:(tb + 1) * 512], start=(kc == 0), stop=(kc == KC - 1)),
                                    reads=[w_b] + hTb[tb * 4:(tb + 1) * 4], writes=[pb[bank]], inc=(kc == KC - 1))
                            if kind == "f":
                                e_t, e_b, e_ch = ev32.next()
                                if (sub + tb) % 2 == 0:
                                    S.op("act", lambda e_t=e_t, bank=bank: nc.scalar.copy(out=e_t[:], in_=ps[bank][:]),
                                         reads=[pb[bank]], writes=[e_b])
                                else:
                                    S.op("dve", lambda e_t=e_t, bank=bank: nc.vector.tensor_copy(out=e_t[:], in_=ps[bank][:]),
                                         reads=[pb[bank]], writes=[e_b])
                                ch0 = c0 - 8192 + sub * 128
                                S.dma("sp", lambda e_t=e_t, ch0=ch0, tb=tb: nc.sync.dma_start(
                                    out=xbcT_d[ch0:ch0 + 128, tok0 + tb * 512: tok0 + (tb + 1) * 512], in_=e_t[:]),
                                    e_ch, reads=[e_b])
                            else:
                                head = ((c0 - (0 if kind == "q" else 2048)) // 128) + sub
                                pending.append(qk_post(kind, head, tb, bank))
                            step_pending()
                else:
                    for tt in range(NT):
                        bank = mainbank[0] % 4
                        mainbank[0] += 1
                        for kc in range(KC):
                            S.op("pe", lambda bank=bank, kc=kc, tt=tt, w_t=w_t, ncol=ncol: nc.tensor.matmul(
                                ps[bank][:, 0:ncol], lhsT=hT[:, kc, tt * 128:(tt + 1) * 128], rhs=w_t[:, kc, 0:ncol],
                                start=(kc == 0), stop=(kc == KC - 1)),
                                reads=[w_b, hTb[tt]], writes=[pb[bank]], inc=(kc == KC - 1))
                        r0 = tok0 + tt * 128
                        if kind == "dt":
                            e_t, e_b, e_ch = ev32.next()
                            S.op("dve", lambda e_t=e_t, bank=bank: nc.vector.tensor_copy(out=e_t[:, 0:32], in_=ps[bank][:, 0:32]),
                                 reads=[pb[bank]], writes=[e_b])
                            S.dma("sp", lambda e_t=e_t, r0=r0: nc.sync.dma_start(out=dtr_d[r0:r0 + 128, :], in_=e_t[:, 0:32]),
                                  e_ch, reads=[e_b])
                        elif kind == "v":
                            o_t, o_b, o_ch = evb.next()
                            if tt % 2 == 0:
                                S.op("act", lambda o_t=o_t, bank=bank: nc.scalar.copy(out=o_t[:], in_=ps[bank][:]),
                                     reads=[pb[bank]], writes=[o_b])
                            else:
                                S.op("dve", lambda o_t=o_t, bank=bank: nc.vector.tensor_copy(out=o_t[:], in_=ps[bank][:]),
                                     reads=[pb[bank]], writes=[o_b])
                            S.dma("sp", lambda o_t=o_t, r0=r0, c0=c0: nc.sync.dma_start(
                                out=v_d[r0:r0 + 128, c0 - 4096:c0 - 4096 + 512], in_=o_t[:]), o_ch, reads=[o_b])
                        else:
                            o_t, o_b, o_ch = evb.next()
                            S.op("act", lambda o_t=o_t, bank=bank: nc.scalar.activation(out=o_t[:], in_=ps[bank][:], func=AF.Silu),
                                 reads=[pb[bank]], writes=[o_b])
                            S.dma("sp", lambda o_t=o_t, tt=tt, c0=c0: nc.sync.dma_start(
                                out=zs_d[tt * 128:(tt + 1) * 128, c0 - 6144:c0 - 6144 + 512], in_=o_t[:]), o_ch, reads=[o_b])
                        step_pending()
            step_pending(flush=True)
            S.barrier()

    NCV_CHUNKS = dbg.get("conv_chunks", 90) if need_moe else 0
    ncv_e = (NCV_CHUNKS + 5) // 6
    conv_state = {"next": 0}
    conv_bufs = {}
    if NCV_CHUNKS:
        wbg_d = dscr("s_wbg", [ncv_e, D, FF], BF16)
        wbu_d = dscr("s_wbu", [ncv_e, D, FF], BF16)
        wbd_d = dscr("s_wbd", [ncv_e, FF, D], BF16)
        conv_ch = S.chan()
        S.bg_chans.add(conv_ch)
        conv_order = [(e, k, h) for e in range(ncv_e) for (k, h) in (("g", 0), ("u", 0), ("g", 1), ("u", 1), ("d", 0), ("d", 1))][:NCV_CHUNKS]

    def bg_convert(n):
        for _ in range(n):
            i = conv_state["next"]
            if i >= NCV_CHUNKS:
                return
            conv_state["next"] = i + 1
            e, k, h = conv_order[i]
            if k == "d":
                src = w_down[e][:, h * 2048:(h + 1) * 2048].rearrange("(p r) c -> p r c", p=128)
                dst = wbd_d[e][:, h * 2048:(h + 1) * 2048].rearrange("(p r) c -> p r c", p=128)
            else:
                wsrc, wdst = (w_gate, wbg_d) if k == "g" else (w_up, wbu_d)
                src = wsrc[e][:, h * 512:(h + 1) * 512].rearrange("(p r) c -> p r c", p=128)
                dst = wdst[e][:, h * 512:(h + 1) * 512].rearrange("(p r) c -> p r c", p=128)
            b = Buf("conv%d" % i)
            conv_bufs[(e, k, h)] = b
            S.dma("pool", lambda src=src, dst=dst: nc.gpsimd.dma_start(out=dst, in_=src), conv_ch, writes=[b])

    def transpose_rows(src_t, src_b, dstT, dst_b, tt, evac_alt=0):
        for q4 in range(4):
            bank = q4
            pbf = ps[bank][:].bitcast(BF16)
            for j in range(8):
                kc = q4 * 8 + j
                S.op("pe", lambda pbf=pbf, j=j, kc=kc: nc.tensor.transpose(
                    out=pbf[:, j * 128:(j + 1) * 128], in_=src_t[:, kc * 128:(kc + 1) * 128], identity=ident_b[:]),
                    reads=[src_b, CB], writes=[pb[bank]], inc=(j == 7))
            src = pbf.rearrange("p (j t) -> p j t", j=8)
            dst = dstT[:, q4 * 8:(q4 + 1) * 8, tt * 128:(tt + 1) * 128]
            if (q4 + evac_alt) % 2 == 0:
                S.op("act", lambda dst=dst, src=src: nc.scalar.copy(out=dst, in_=src), reads=[pb[bank]], writes=[dst_b])
            else:
                S.op("dve", lambda dst=dst, src=src: nc.vector.tensor_copy(out=dst, in_=src), reads=[pb[bank]], writes=[dst_b])

    def attention():
        with ExitStack() as st:
            kTr = Rot(S, st, "kTh", [128, 2 * NTOK], BF16, 2, dma=True)
            qTr = Rot(S, st, "qTh", [128, NTOK], BF16, 2, dma=True)
            vr = Rot(S, st, "vh", [128, 16, 129], BF16, 2, dma=True)
            per = Rot(S, st, "pexp", [128, 512], F32, 3)
            ptr = Rot(S, st, "pT", [128, 512], BF16, 4)
            SBANK = (0, 1, 6, 7)
            otr = Rot(S, st, "ot", [128, NT, 128], BF16, 2, dma=True)
            rcr = Rot(S, st, "rc", [128, 1], F32, 4)
            for i in range(2):
                S.op("dve", lambda i=i: nc.vector.memset(vr.t[i][:, NT:16, 128:129], 1.0), writes=[vr.b[i]])
                S.op("dve", lambda i=i: nc.vector.tensor_copy(out=vr.t[i][:, 0:NT, 128:129], in_=vflag[:, :].unsqueeze(2)),
                     reads=[CB], writes=[vr.b[i]])

            def loads(h):
                k_t, k_b, k_ch = kTr.next()
                q_t, q_b, q_ch = qTr.next()
                v_t, v_b, v_ch = vr.next()
                S.dma("sp", lambda: nc.sync.dma_start(out=k_t[:], in_=kT_d[h]), k_ch, writes=[k_b])
                S.dma("sp", lambda: nc.sync.dma_start(out=q_t[:], in_=qT_d[h]), q_ch, writes=[q_b])
                S.dma("sp", lambda: nc.sync.dma_start(
                    out=v_t[:, :, 0:128], in_=v_d[:, h * 128:(h + 1) * 128].rearrange("(t p) d -> p t d", p=128)),
                    v_ch, writes=[v_b])
                return (k_t, k_b, q_t, q_b, v_t, v_b)

            bg_convert(42)
            nxt = loads(0)
            heads = dbg.get("heads", NH)
            for h in range(heads):
                k_t, k_b, q_t, q_b, v_t, v_b = nxt
                if h + 1 < heads:
                    nxt = loads(h + 1)
                o_t, o_b, o_ch = otr.next()
                for c in range(2):
                    nkb = NT + 4 * c + 4

                    def s_mm(kb):
                        js = max(0, kb - NT - 4 * c)
                        bank = SBANK[kb % 4]
                        S.op("pe", lambda: nc.tensor.matmul(
                            ps[bank][:, js * 128:512], lhsT=k_t[:, kb * 128:(kb + 1) * 128],
                            rhs=q_t[:, c * 512 + js * 128:(c + 1) * 512], start=True, stop=True),
                            reads=[k_b, q_b], writes=[pb[bank]])
                    for k0 in range(min(3, nkb)):
                        s_mm(k0)
                    for kb in range(nkb):
                        if kb + 3 < nkb:
                            s_mm(kb + 3)
                        js = max(0, kb - NT - 4 * c)
                        bank = SBANK[kb % 4]
                        pe_t, pe_b = per.next()
                        S.op("act", lambda: nc.scalar.activation(out=pe_t[:, js * 128:512], in_=ps[bank][:, js * 128:512],
                                                                 func=AF.Exp), reads=[pb[bank]], writes=[pe_b])
                        pt_t, pt_b = ptr.next()
                        d0 = (NT + 4 * c + js) - kb
                        S.op("dve", lambda: nc.vector.tensor_tensor(
                            out=pt_t[:, js * 128:512], in0=pe_t[:, js * 128:512],
                            in1=amask[:, d0 * 128:(d0 + 4 - js) * 128], op=ALU.mult), reads=[pe_b, CB], writes=[pt_b])
                        for j in range(js, 4):
                            qb = NT + 4 * c + j
                            S.op("pe", lambda j=j, qb=qb: nc.tensor.matmul(
                                ps[2 + j][:, 0:129], lhsT=pt_t[:, j * 128:(j + 1) * 128], rhs=v_t[:, kb, :],
                                start=(kb == 0), stop=(kb == qb)), reads=[pt_b, v_b], writes=[pb[2 + j]])
                    for j in range(4):
                        rc_t, rc_b = rcr.next()
                        S.op("dve", lambda j=j, rc_t=rc_t: nc.vector.reciprocal(out=rc_t[:], in_=ps[2 + j][:, 128:129]),
                             reads=[pb[2 + j]], writes=[rc_b])
                        S.op("dve", lambda j=j, rc_t=rc_t: nc.vector.tensor_scalar(
                            out=o_t[:, 4 * c + j, :], in0=ps[2 + j][:, 0:128], scalar1=rc_t[:, 0:1], scalar2=None,
                            op0=ALU.mult), reads=[pb[2 + j], rc_b], writes=[o_b])
                S.dma("sp", lambda: nc.sync.dma_start(
                    out=mix_d[:, h * 128:(h + 1) * 128].rearrange("(t p) d -> p t d", p=128), in_=o_t[:]),
                    o_ch, reads=[o_b])
            S.barrier()

    def ssd():
        T2 = 2 * NTOK
        with ExitStack() as st:
            dt_all = sb(st, "dt_all", [128, 16, 32], F32)
            dA_all = sb(st, "dA_all", [128, 16, 32], F32)
            dtv = sb(st, "dtv", [128, 16, 32], F32)
            bbc = sb(st, "bbc", [128, 32], F32)
            abc = sb(st, "abc", [128, 32], F32)
            vcol = sb(st, "vcol", [128, 16], F32)
            cw = sb(st, "cw", [128, 32, 4], F32)
            cbias = sb(st, "cbias", [128, 32], F32)
            d_all = sb(st, "d_all", [128, 2048], F32)
            nw_all = sb(st, "nw_all", [128, 2048], F32)
            PB = Buf("ssd_params")
            pch = S.chan()
            S.dma("sp", lambda: nc.sync.dma_start(out=dt_all[:], in_=dtr_d.rearrange("(t p) h -> p t h", p=128)), pch, writes=[PB])
            S.dma("sp", lambda: nc.sync.dma_start(out=bbc[:], in_=dt_bias.partition_broadcast(128)), pch, writes=[PB])
            S.dma("sp", lambda: nc.sync.dma_start(out=abc[:], in_=a_log.partition_broadcast(128)), pch, writes=[PB])
            S.dma("sp", lambda: nc.sync.dma_start(out=cw[:], in_=conv_wc), pch, writes=[PB])
            S.dma("sp", lambda: nc.sync.dma_start(out=cbias[:], in_=conv_bc), pch, writes=[PB])
            S.dma("sp", lambda: nc.sync.dma_start(out=d_all[:], in_=d_rep.partition_broadcast(128)), pch, writes=[PB])
            S.dma("sp", lambda: nc.sync.dma_start(out=nw_all[:], in_=ssd_nw.partition_broadcast(128)), pch, writes=[PB])
            S.op("dve", lambda: nc.vector.tensor_tensor(out=dt_all[:], in0=dt_all[:],
                                                        in1=bbc[:, :].unsqueeze(1).to_broadcast([128, 16, 32]), op=ALU.add),
                 reads=[PB], writes=[PB])
            S.op("act", lambda: nc.scalar.activation(out=dt_all[:], in_=dt_all[:], func=AF.Exp), reads=[PB], writes=[PB])
            S.op("act", lambda: nc.scalar.activation(out=dt_all[:], in_=dt_all[:], func=AF.Ln, bias=ones_f[:, 0:1]),
                 reads=[PB, CB], writes=[PB])
            S.op("act", lambda: nc.scalar.activation(out=abc[:], in_=abc[:], func=AF.Exp), reads=[PB], writes=[PB])
            S.op("dve", lambda: nc.vector.scalar_tensor_tensor(
                out=dA_all[:], in0=dt_all[:], scalar=-1.0, in1=abc[:, :].unsqueeze(1).to_broadcast([128, 16, 32]),
                op0=ALU.mult, op1=ALU.mult), reads=[PB], writes=[PB])
            S.op("dve", lambda: nc.vector.memset(vcol[:, NT:16], 1.0), writes=[PB])
            S.op("dve", lambda: nc.vector.tensor_copy(out=vcol[:, 0:NT], in_=vflag[:, :]), reads=[CB], writes=[PB])
            S.op("dve", lambda: nc.vector.tensor_tensor(out=dtv[:], in0=dt_all[:],
                                                        in1=vcol[:, :].unsqueeze(2).to_broadcast([128, 16, 32]), op=ALU.mult),
                 reads=[PB], writes=[PB])

            lvl = dbg.get("ssd_level", 99)
            if lvl <= 1:
                S.barrier()
                return
            xraw = sb(st, "xraw", [128, 2, 3 + T2], F32)
            bcraw = sb(st, "bcraw", [128, 2, 3 + T2], F32)
            RAW = [Buf("raw%d" % i) for i in range(4)]
            rch = [S.chan() for _ in range(4)]
            S.op("dve", lambda: nc.vector.memset(xraw[:, :, 0:3], 0.0), writes=RAW[0:2])
            S.op("dve", lambda: nc.vector.memset(bcraw[:, :, 0:3], 0.0), writes=RAW[2:4])
            acc = Rot(S, st, "cacc", [128, T2], F32, 2)
            xTa = sb(st, "xTa", [128, 4, T2], BF16)
            XA = [Buf("xTa%d" % i) for i in range(4)]
            x_tok = sb(st, "x_tok", [128, 16, 256], BF16)
            b_tok = sb(st, "b_tok", [128, 16, 128], BF16)
            xdt = sb(st, "xdt", [128, 16, 256], BF16)
            XT = Buf("x_tok")
            BT = Buf("b_tok")
            XD = Buf("xdt")
            hst = sb(st, "hst", [128, 256], F32)
            hbf = sb(st, "hbf", [128, 256], BF16)
            HS = Buf("hst")
            HB = Buf("hbf")
            rhsr = Rot(S, st, "rhsH", [128, 128], F32, 3)
            segr = Rot(S, st, "segT", [128, 128], F32, 3)
            gtr = Rot(S, st, "GT", [128, 128], BF16, 3)
            dqr = Rot(S, st, "decq", [128, 4], F32, 2)
            csr = Rot(S, st, "ncs", [128, 64], F32, 2)
            cbr = Rot(S, st, "cbt", [128, 128], F32, 2)
            xwr = Rot(S, st, "xdtw", [128, 256], BF16, 2)
            ydr = Rot(S, st, "ydsb", [128, 256], F32, 2)
            yr = Rot(S, st, "yy", [128, 256], F32, 2)
            tmr = Rot(S, st, "ytmp", [128, 256], F32, 2)
            zr = Rot(S, st, "zst", [128, 256], BF16, 2, dma=True)
            yor = Rot(S, st, "yo", [128, 256], BF16, 2, dma=True)
            s2r = Rot(S, st, "ss2", [128, 2], F32, 2)
            jk = sb(st, "jk2", [128, 256], BF16)
            JK = Buf("jk2")
            groups = dbg.get("groups", 8)
            for g in range(groups):
                srcs = [(xraw, 0, g * 256, 2 * g), (xraw, 1, g * 256 + 128, 2 * g + 1),
                        (bcraw, 0, 2048 + g * 128, 16 + g), (bcraw, 1, 3072 + g * 128, 24 + g)]
                for i, (rt, ri, ch0, blk) in enumerate(srcs):
                    S.dma("sp", lambda rt=rt, ri=ri, ch0=ch0: nc.sync.dma_start(out=rt[:, ri, 3:3 + T2], in_=xbcT_d[ch0:ch0 + 128, :]),
                          rch[i], writes=[RAW[i]])
                for i, (rt, ri, ch0, blk) in enumerate(srcs):
                    a_t, a_b = acc.next()
                    S.op("dve", lambda rt=rt, ri=ri, blk=blk, a_t=a_t: nc.vector.tensor_scalar(
                        out=a_t[:], in0=rt[:, ri, 0:T2], scalar1=cw[:, blk, 0:1], scalar2=None, op0=ALU.mult),
                        reads=[RAW[i], PB], writes=[a_b])
                    for k in range(1, 4):
                        S.op("dve", lambda rt=rt, ri=ri, blk=blk, a_t=a_t, k=k: nc.vector.scalar_tensor_tensor(
                            out=a_t[:], in0=rt[:, ri, k:k + T2], scalar=cw[:, blk, k:k + 1], in1=a_t[:],
                            op0=ALU.mult, op1=ALU.add), reads=[RAW[i], PB, a_b], writes=[a_b])
                    S.op("act", lambda i=i, blk=blk, a_t=a_t: nc.scalar.activation(
                        out=xTa[:, i, :], in_=a_t[:], func=AF.Silu, bias=cbias[:, blk:blk + 1]),
                        reads=[a_b, PB], writes=[XA[i]])
                if lvl <= 2:
                    continue
                for t in range(16):
                    bank = 6 + t % 2
                    pbf = ps[bank][:].bitcast(BF16)
                    for i in range(3):
                        S.op("pe", lambda pbf=pbf, i=i, t=t: nc.tensor.transpose(
                            out=pbf[:, i * 128:(i + 1) * 128], in_=xTa[:, i, t * 128:(t + 1) * 128], identity=ident_b[:]),
                            reads=[XA[i], CB], writes=[pb[bank]], inc=(i == 2))
                    S.op("act", lambda pbf=pbf, t=t: nc.scalar.copy(out=x_tok[:, t, :], in_=pbf[:, 0:256]),
                         reads=[pb[bank]], writes=[XT])
                    S.op("dve", lambda pbf=pbf, t=t: nc.vector.tensor_copy(out=b_tok[:, t, :], in_=pbf[:, 256:384]),
                         reads=[pb[bank]], writes=[BT])
                S.op("dve", lambda g=g: nc.vector.tensor_tensor(
                    out=xdt[:, :, :].rearrange("p t (h d) -> p t h d", h=4),
                    in0=x_tok[:, :, :].rearrange("p t (h d) -> p t h d", h=4),
                    in1=dtv[:, :, 4 * g:4 * g + 4].unsqueeze(3).to_broadcast([128, 16, 4, 64]), op=ALU.mult),
                    reads=[XT, PB], writes=[XD])
                S.op("dve", lambda: nc.vector.memset(hst[:], 0.0), writes=[HS])
                S.op("dve", lambda: nc.vector.memset(hbf[:], 0.0), writes=[HB])
                if lvl <= 3:
                    continue
                for c in range(dbg.get("chunks", 16)):
                    own = c >= NT
                    cs = slice(c * 128, (c + 1) * 128)
                    S.op("pe", lambda c=c: nc.tensor.matmul(ps[2][:, 0:32], lhsT=tri_f[:], rhs=dA_all[:, c, :], start=True, stop=True),
                         reads=[CB, PB], writes=[pb[2]])
                    cs_t, cs_b = csr.next()
                    S.op("act", lambda cs_t=cs_t: nc.scalar.activation(out=cs_t[:, 0:32], in_=ps[2][:, 0:32], func=AF.Copy, scale=-1.0),
                         reads=[pb[2]], writes=[cs_b])
                    if own:
                        S.op("act", lambda cs_t=cs_t: nc.scalar.activation(out=cs_t[:, 32:64], in_=ps[2][:, 0:32], func=AF.Exp),
                             reads=[pb[2]], writes=[cs_b])
                        S.op("pe", lambda cs=cs: nc.tensor.matmul(ps[3][:, 0:128], lhsT=xTa[:, 2, cs], rhs=xTa[:, 3, cs], start=True, stop=True),
                             reads=[XA[2], XA[3]], writes=[pb[3]])
                        cb_t, cb_b = cbr.next()
                        S.op("dve", lambda cb_t=cb_t: nc.vector.tensor_copy(out=cb_t[:], in_=ps[3][:, 0:128]), reads=[pb[3]], writes=[cb_b])
                        S.op("pe", lambda cs=cs: nc.tensor.matmul(ps[4][:, 0:256], lhsT=xTa[:, 3, cs], rhs=hbf[:], start=True, stop=True),
                             reads=[XA[3], HB], writes=[pb[4]])
                    dq_t, dq_b = dqr.next()
                    xw_t, xw_b = xwr.next()
                    for hh in range(4):
                        H = 4 * g + hh
                        r_t, r_b = rhsr.next()
                        S.op("dve", lambda r_t=r_t, c=c, H=H: nc.vector.tensor_scalar(
                            out=r_t[:], in0=tri_f[:], scalar1=dA_all[:, c, H:H + 1], scalar2=None, op0=ALU.mult),
                            reads=[CB, PB], writes=[r_b])
                        bank = hh % 2
                        S.op("pe", lambda r_t=r_t, bank=bank: nc.tensor.matmul(ps[bank][:, 0:128], lhsT=ones_f[:], rhs=r_t[:], start=True, stop=False),
                             reads=[r_b, CB], writes=[pb[bank]], inc=False)
                        S.op("pe", lambda bank=bank: nc.tensor.matmul(ps[bank][:, 0:128], lhsT=ident_f[:], rhs=negm_f[:], start=False, stop=True),
                             reads=[CB], writes=[pb[bank]])
                        sg_t, sg_b = segr.next()
                        S.op("act", lambda sg_t=sg_t, bank=bank, cs_t=cs_t, H=H: nc.scalar.activation(
                            out=sg_t[:], in_=ps[bank][:, 0:128], func=AF.Exp, bias=cs_t[:, H:H + 1]),
                            reads=[pb[bank], cs_b], writes=[sg_b])
                        S.op("act", lambda dq_t=dq_t, bank=bank, hh=hh: nc.scalar.activation(
                            out=dq_t[:, hh:hh + 1], in_=ps[bank][:, 127:128], func=AF.Exp), reads=[pb[bank]], writes=[dq_b])
                        if own:
                            g_t, g_b = gtr.next()
                            S.op("dve", lambda g_t=g_t, cb_t=cb_t, sg_t=sg_t: nc.vector.tensor_tensor(
                                out=g_t[:], in0=cb_t[:], in1=sg_t[:], op=ALU.mult), reads=[cb_b, sg_b], writes=[g_b])
                            S.op("pe", lambda g_t=g_t, hh=hh, c=c: nc.tensor.matmul(
                                ps[5][:, hh * 64:(hh + 1) * 64], lhsT=g_t[:], rhs=xdt[:, c, hh * 64:(hh + 1) * 64], start=True, stop=True),
                                reads=[g_b, XD], writes=[pb[5]])
                        S.op("dve", lambda xw_t=xw_t, sg_t=sg_t, hh=hh, c=c: nc.vector.tensor_scalar(
                            out=xw_t[:, hh * 64:(hh + 1) * 64], in0=xdt[:, c, hh * 64:(hh + 1) * 64], scalar1=sg_t[:, 127:128],
                            scalar2=None, op0=ALU.mult), reads=[XD, sg_b], writes=[xw_b])
                    S.op("pe", lambda xw_t=xw_t, c=c: nc.tensor.matmul(ps[6][:, 0:256], lhsT=b_tok[:, c, :], rhs=xw_t[:], start=True, stop=True),
                         reads=[BT, xw_b], writes=[pb[6]])
                    if own:
                        yd_t, yd_b = ydr.next()
                        S.op("act", lambda yd_t=yd_t: nc.scalar.copy(out=yd_t[:], in_=ps[5][:, 0:256]), reads=[pb[5]], writes=[yd_b])
                        y_t, y_b = yr.next()
                        for hh in range(4):
                            H = 4 * g + hh
                            S.op("dve", lambda y_t=y_t, yd_t=yd_t, hh=hh, H=H, cs_t=cs_t: nc.vector.scalar_tensor_tensor(
                                out=y_t[:, hh * 64:(hh + 1) * 64], in0=ps[4][:, hh * 64:(hh + 1) * 64], scalar=cs_t[:, 32 + H:33 + H],
                                in1=yd_t[:, hh * 64:(hh + 1) * 64], op0=ALU.mult, op1=ALU.add),
                                reads=[pb[4], cs_b, yd_b], writes=[y_b])
                    for hh in range(4):
                        S.op("dve", lambda hh=hh, dq_t=dq_t: nc.vector.scalar_tensor_tensor(
                            out=hst[:, hh * 64:(hh + 1) * 64], in0=hst[:, hh * 64:(hh + 1) * 64], scalar=dq_t[:, hh:hh + 1],
                            in1=ps[6][:, hh * 64:(hh + 1) * 64], op0=ALU.mult, op1=ALU.add),
                            reads=[HS, dq_b, pb[6]], writes=[HS])
                    S.op("act", lambda: nc.scalar.copy(out=hbf[:], in_=hst[:]), reads=[HS], writes=[HB])
                    if own:
                        r0 = (c - NT) * 128
                        z_t, z_b, z_ch = zr.next()
                        S.dma("sp", lambda z_t=z_t, r0=r0, g=g: nc.sync.dma_start(out=z_t[:], in_=zs_d[r0:r0 + 128, g * 256:(g + 1) * 256]),
                              z_ch, writes=[z_b])
                        tm_t, tm_b = tmr.next()
                        S.op("dve", lambda tm_t=tm_t, c=c, g=g: nc.vector.tensor_tensor(
                            out=tm_t[:], in0=x_tok[:, c, :], in1=d_all[:, g * 256:(g + 1) * 256], op=ALU.mult),
                            reads=[XT, PB], writes=[tm_b])
                        S.op("dve", lambda tm_t=tm_t, y_t=y_t: nc.vector.tensor_tensor(out=y_t[:], in0=y_t[:], in1=tm_t[:], op=ALU.add),
                             reads=[tm_b, y_b], writes=[y_b])
                        S.op("dve", lambda z_t=z_t, y_t=y_t: nc.vector.tensor_tensor(out=y_t[:], in0=y_t[:], in1=z_t[:], op=ALU.mult),
                             reads=[z_b, y_b], writes=[y_b])
                        s_t, s_b = s2r.next()
                        S.op("act", lambda y_t=y_t, s_t=s_t: nc.scalar.activation(out=jk[:], in_=y_t[:], func=AF.Square, accum_out=s_t[:, 0:1]),
                             reads=[y_b], writes=[JK, s_b])
                        S.op("act", lambda s_t=s_t: nc.scalar.activation(out=s_t[:, 1:2], in_=s_t[:, 0:1], func=AF.Sqrt, scale=1.0 / 256,
                                                                         bias=eps5[:, 0:1]), reads=[s_b, CB], writes=[s_b])
                        S.op("dve", lambda s_t=s_t: nc.vector.reciprocal(out=s_t[:, 1:2], in_=s_t[:, 1:2]), reads=[s_b], writes=[s_b])
                        yo_t, yo_b, yo_ch = yor.next()
                        S.op("dve", lambda yo_t=yo_t, y_t=y_t, s_t=s_t, g=g: nc.vector.scalar_tensor_tensor(
                            out=yo_t[:], in0=y_t[:], scalar=s_t[:, 1:2], in1=nw_all[:, g * 256:(g + 1) * 256],
                            op0=ALU.mult, op1=ALU.mult), reads=[y_b, s_b, PB], writes=[yo_b])
                        S.dma("sp", lambda yo_t=yo_t, r0=r0, g=g: nc.sync.dma_start(
                            out=mix_d[r0:r0 + 128, 2048 + g * 256:2048 + (g + 1) * 256], in_=yo_t[:]), yo_ch, reads=[yo_b])
            S.barrier()

    def ssd2():
        T2 = 2 * NTOK
        with ExitStack() as st:
            dt_all = sb(st, "dt_all", [128, 16, 32], F32)
            dA_all = sb(st, "dA_all", [128, 16, 32], F32)
            dtv = sb(st, "dtv", [128, 16, 32], F32)
            ncs_all = sb(st, "ncs_all", [128, 16, 32], F32)
            ecs_all = sb(st, "ecs_all", [128, 16, 32], F32)
            decq_all = sb(st, "decq_all", [128, 16, 32], F32)
            w2_all = sb(st, "w2_all", [128, 16, 32], F32)
            bbc = sb(st, "bbc", [128, 32], F32)
            abc = sb(st, "abc", [128, 32], F32)
            vcol = sb(st, "vcol", [128, 16], F32)
            cw = sb(st, "cw", [128, 32, 4], F32)
            cbias = sb(st, "cbias", [128, 32], F32)
            d_all = sb(st, "d_all", [128, 2048], F32)
            nw_all = sb(st, "nw_all", [128, 2048], F32)
            negm4 = sb(st, "negm4", [128, 4, 128], F32)
            PB = Buf("ssd_params")
            pch = S.chan()
            S.dma("sp", lambda: nc.sync.dma_start(out=dt_all[:], in_=dtr_d.rearrange("(t p) h -> p t h", p=128)), pch, writes=[PB])
            S.dma("sp", lambda: nc.sync.dma_start(out=bbc[:], in_=dt_bias.partition_broadcast(128)), pch, writes=[PB])
            S.dma("sp", lambda: nc.sync.dma_start(out=abc[:], in_=a_log.partition_broadcast(128)), pch, writes=[PB])
            S.dma("sp", lambda: nc.sync.dma_start(out=cw[:], in_=conv_wc), pch, writes=[PB])
            S.dma("sp", lambda: nc.sync.dma_start(out=cbias[:], in_=conv_bc), pch, writes=[PB])
            S.dma("sp", lambda: nc.sync.dma_start(out=d_all[:], in_=d_rep.partition_broadcast(128)), pch, writes=[PB])
            S.dma("sp", lambda: nc.sync.dma_start(out=nw_all[:], in_=ssd_nw.partition_broadcast(128)), pch, writes=[PB])
            S.op("dve", lambda: nc.vector.tensor_tensor(out=dt_all[:], in0=dt_all[:],
                                                        in1=bbc[:, :].unsqueeze(1).to_broadcast([128, 16, 32]), op=ALU.add),
                 reads=[PB], writes=[PB])
            S.op("act", lambda: nc.scalar.activation(out=dt_all[:], in_=dt_all[:], func=AF.Exp), reads=[PB], writes=[PB])
            S.op("act", lambda: nc.scalar.activation(out=dt_all[:], in_=dt_all[:], func=AF.Ln, bias=ones_f[:, 0:1]),
                 reads=[PB, CB], writes=[PB])
            S.op("act", lambda: nc.scalar.activation(out=abc[:], in_=abc[:], func=AF.Exp), reads=[PB], writes=[PB])
            S.op("dve", lambda: nc.vector.scalar_tensor_tensor(
                out=dA_all[:], in0=dt_all[:], scalar=-1.0, in1=abc[:, :].unsqueeze(1).to_broadcast([128, 16, 32]),
                op0=ALU.mult, op1=ALU.mult), reads=[PB], writes=[PB])
            S.op("dve", lambda: nc.vector.memset(vcol[:, NT:16], 1.0), writes=[PB])
            S.op("dve", lambda: nc.vector.tensor_copy(out=vcol[:, 0:NT], in_=vflag[:, :]), reads=[CB], writes=[PB])
            S.op("dve", lambda: nc.vector.tensor_tensor(out=dtv[:], in0=dt_all[:],
                                                        in1=vcol[:, :].unsqueeze(2).to_broadcast([128, 16, 32]), op=ALU.mult),
                 reads=[PB], writes=[PB])
            S.op("dve", lambda: nc.vector.tensor_copy(out=negm4[:], in_=negm_f[:, :].unsqueeze(1).to_broadcast([128, 4, 128])),
                 reads=[CB], writes=[PB])
            flat = lambda t: t[:, :, :].rearrange("p c h -> p (c h)")
            S.op("pe", lambda: nc.tensor.matmul(ps[2][:], lhsT=tri_f[:], rhs=flat(dA_all), start=True, stop=True),
                 reads=[CB, PB], writes=[pb[2]])
            S.op("pe", lambda: nc.tensor.matmul(ps[3][:], lhsT=ones_f[:], rhs=flat(dA_all), start=True, stop=True),
                 reads=[CB, PB], writes=[pb[3]])
            S.op("act", lambda: nc.scalar.activation(out=flat(ncs_all), in_=ps[2][:], func=AF.Copy, scale=-1.0),
                 reads=[pb[2]], writes=[PB])
            S.op("act", lambda: nc.scalar.activation(out=flat(ecs_all), in_=ps[2][:], func=AF.Exp), reads=[pb[2]], writes=[PB])
            S.op("act", lambda: nc.scalar.activation(out=flat(decq_all), in_=ps[3][:], func=AF.Exp), reads=[pb[3]], writes=[PB])
            S.op("dve", lambda: nc.vector.tensor_tensor(out=flat(w2_all), in0=ps[3][:], in1=flat(ncs_all), op=ALU.add),
                 reads=[pb[3], PB], writes=[PB])
            S.op("act", lambda: nc.scalar.activation(out=flat(w2_all), in_=flat(w2_all), func=AF.Exp), reads=[PB], writes=[PB])
            S.op("dve", lambda: nc.vector.tensor_tensor(out=flat(w2_all), in0=flat(w2_all), in1=flat(dtv), op=ALU.mult),
                 reads=[PB], writes=[PB])

            xraw = sb(st, "xraw", [128, 2, 3 + T2], F32)
            bcraw = sb(st, "bcraw", [128, 2, 3 + T2], F32)
            RAW = [Buf("raw%d" % i) for i in range(4)]
            rch = [S.chan() for _ in range(4)]
            S.op("dve", lambda: nc.vector.memset(xraw[:, :, 0:3], 0.0), writes=RAW[0:2])
            S.op("dve", lambda: nc.vector.memset(bcraw[:, :, 0:3], 0.0), writes=RAW[2:4])
            acc = Rot(S, st, "cacc", [128, T2], F32, 2)
            xTa = sb(st, "xTa", [128, 4, T2], BF16)
            XA = [Buf("xTa%d" % i) for i in range(4)]
            x_tok = sb(st, "x_tok", [128, 16, 256], BF16)
            b_tok = sb(st, "b_tok", [128, 16, 128], BF16)
            xdt = sb(st, "xdt", [128, NT, 256], BF16)
            xdtw = sb(st, "xdtw_all", [128, 16, 256], BF16)
            XT = Buf("x_tok")
            BT = Buf("b_tok")
            XD = Buf("xdt")
            XW = Buf("xdtw")
            S_all = sb(st, "S_all", [128, 16, 256], F32)
            SB_ = [Buf("S%d" % i) for i in range(16)]
            hbf_all = sb(st, "hbf_all", [128, NT, 256], BF16)
            HB = Buf("hbf_all")
            htmp = Rot(S, st, "htmp", [128, 256], F32, 2)
            y_all = sb(st, "y_all", [128, NT, 256], F32)
            YB = [Buf("y%d" % i) for i in range(NT)]
            dx_all = sb(st, "dx_all", [128, NT, 256], F32)
            DX = Buf("dx_all")
            zs_g = sb(st, "zs_g", [128, NT, 256], BF16)
            ZG = Buf("zs_g")
            zch = S.chan()
            yo_g = sb(st, "yo_g", [128, NT, 256], BF16)
            YO = Buf("yo_g")
            ych = S.chan()
            ssg = sb(st, "ssg", [128, 2 * NT], F32)
            SG = Buf("ssg")
            rhsr = Rot(S, st, "rhsH4", [128, 4, 128], F32, 2)
            segr = Rot(S, st, "segT4", [128, 4, 128], F32, 2)
            gtr = Rot(S, st, "GT4", [128, 4, 128], BF16, 2)
            cbr = Rot(S, st, "cbt", [128, 128], F32, 2)
            ydr = Rot(S, st, "ydsb", [128, 256], F32, 2)
            groups = dbg.get("groups", 8)
            for g in range(groups):
                hs4 = slice(4 * g, 4 * g + 4)
                srcs = [(xraw, 0, g * 256, 2 * g), (xraw, 1, g * 256 + 128, 2 * g + 1),
                        (bcraw, 0, 2048 + g * 128, 16 + g), (bcraw, 1, 3072 + g * 128, 24 + g)]
                for i, (rt, ri, ch0, blk) in enumerate(srcs):
                    S.dma("sp", lambda rt=rt, ri=ri, ch0=ch0: nc.sync.dma_start(out=rt[:, ri, 3:3 + T2], in_=xbcT_d[ch0:ch0 + 128, :]),
                          rch[i], writes=[RAW[i]])
                S.dma("sp", lambda g=g: nc.sync.dma_start(
                    out=zs_g[:], in_=zs_d[:, g * 256:(g + 1) * 256].rearrange("(t p) d -> p t d", p=128)), zch, writes=[ZG])
                for i, (rt, ri, ch0, blk) in enumerate(srcs):
                    a_t, a_b = acc.next()
                    E = "dve"
                    eng = nc.vector
                    S.op(E, lambda rt=rt, ri=ri, blk=blk, a_t=a_t, eng=eng: eng.tensor_scalar(
                        out=a_t[:], in0=rt[:, ri, 0:T2], scalar1=cw[:, blk, 0:1], scalar2=None, op0=ALU.mult),
                        reads=[RAW[i], PB], writes=[a_b])
                    for k in range(1, 4):
                        if E == "dve":
                            S.op(E, lambda rt=rt, ri=ri, blk=blk, a_t=a_t, k=k, eng=eng: eng.scalar_tensor_tensor(
                                out=a_t[:], in0=rt[:, ri, k:k + T2], scalar=cw[:, blk, k:k + 1], in1=a_t[:],
                                op0=ALU.mult, op1=ALU.add), reads=[RAW[i], PB, a_b], writes=[a_b])
                        else:
                            tmpv = dx_all[:, :, :].rearrange("p t d -> p (t d)")
                            S.op(E, lambda rt=rt, ri=ri, blk=blk, k=k, tmpv=tmpv: nc.gpsimd.tensor_scalar(
                                out=tmpv, in0=rt[:, ri, k:k + T2], scalar1=cw[:, blk, k:k + 1], scalar2=None, op0=ALU.mult),
                                reads=[RAW[i], PB], writes=[DX])
                            S.op(E, lambda a_t=a_t, tmpv=tmpv: nc.gpsimd.tensor_tensor(out=a_t[:], in0=a_t[:], in1=tmpv, op=ALU.add),
                                 reads=[DX, a_b], writes=[a_b])
                    S.op("act", lambda i=i, blk=blk, a_t=a_t: nc.scalar.activation(
                        out=xTa[:, i, :], in_=a_t[:], func=AF.Silu, bias=cbias[:, blk:blk + 1]),
                        reads=[a_b, PB], writes=[XA[i]])
                for t in range(16):
                    bank = 6 + t % 2
                    pbf = ps[bank][:].bitcast(BF16)
                    for i in range(3):
                        S.op("pe", lambda pbf=pbf, i=i, t=t: nc.tensor.transpose(
                            out=pbf[:, i * 128:(i + 1) * 128], in_=xTa[:, i, t * 128:(t + 1) * 128], identity=ident_b[:]),
                            reads=[XA[i], CB], writes=[pb[bank]], inc=(i == 2))
                    S.op("act", lambda pbf=pbf, t=t: nc.scalar.copy(out=x_tok[:, t, :], in_=pbf[:, 0:256]),
                         reads=[pb[bank]], writes=[XT])
                    S.op("act", lambda pbf=pbf, t=t: nc.scalar.copy(out=b_tok[:, t, :], in_=pbf[:, 256:384]),
                         reads=[pb[bank]], writes=[BT])
                v4 = lambda ap: ap.rearrange("p t (h d) -> p t h d", h=4)
                S.op("dve", lambda: nc.vector.tensor_tensor(
                    out=v4(xdtw[:, :, :]), in0=v4(x_tok[:, :, :]),
                    in1=w2_all[:, :, hs4].unsqueeze(3).to_broadcast([128, 16, 4, 64]), op=ALU.mult),
                    reads=[XT, PB], writes=[XW])
                S.op("dve", lambda: nc.vector.tensor_tensor(
                    out=v4(xdt[:, :, :]), in0=v4(x_tok[:, NT:16, :]),
                    in1=dtv[:, NT:16, hs4].unsqueeze(3).to_broadcast([128, NT, 4, 64]), op=ALU.mult),
                    reads=[XT, PB], writes=[XD])
                S.op("dve", lambda g=g: nc.vector.tensor_tensor(
                    out=dx_all[:, :, :], in0=x_tok[:, NT:16, :],
                    in1=d_all[:, g * 256:(g + 1) * 256].unsqueeze(1).to_broadcast([128, NT, 256]), op=ALU.mult),
                    reads=[XT, PB], writes=[DX])
                for c2 in range(8):
                    bank = 6 + c2 % 2
                    ncs_ = 2 if c2 < 7 else 1
                    for u in range(ncs_):
                        c = 2 * c2 + u
                        S.op("pe", lambda c=c, u=u, bank=bank: nc.tensor.matmul(
                            ps[bank][:, u * 256:(u + 1) * 256], lhsT=b_tok[:, c, :], rhs=xdtw[:, c, :], start=True, stop=True),
                            reads=[BT, XW], writes=[pb[bank]])
                    dst = S_all[:, 2 * c2:2 * c2 + ncs_, :]
                    src = ps[bank][:, 0:256 * ncs_].rearrange("p (u d) -> p u d", u=ncs_)
                    if c2 % 2 == 0:
                        S.op("act", lambda dst=dst, src=src: nc.scalar.copy(out=dst, in_=src), reads=[pb[bank]],
                             writes=SB_[2 * c2:2 * c2 + ncs_])
                    else:
                        S.op("dve", lambda dst=dst, src=src: nc.vector.tensor_copy(out=dst, in_=src), reads=[pb[bank]],
                             writes=SB_[2 * c2:2 * c2 + ncs_])
                for c in range(1, 15):
                    h_t, h_b = htmp.next()
                    S.op("dve", lambda c=c, h_t=h_t: nc.vector.tensor_tensor(
                        out=h_t[:, :].rearrange("p (h d) -> p h d", h=4),
                        in0=S_all[:, c - 1, :].rearrange("p (h d) -> p h d", h=4),
                        in1=decq_all[:, c, hs4].unsqueeze(2).to_broadcast([128, 4, 64]), op=ALU.mult),
                        reads=[SB_[c - 1], PB], writes=[h_b])
                    S.op("dve", lambda c=c, h_t=h_t: nc.vector.tensor_tensor(out=S_all[:, c, :], in0=S_all[:, c, :], in1=h_t[:], op=ALU.add),
                         reads=[h_b, SB_[c]], writes=[SB_[c]])
                S.op("act", lambda: nc.scalar.copy(out=hbf_all[:, :, :], in_=S_all[:, NT - 1:2 * NT - 1, :]),
                     reads=SB_[NT - 1:2 * NT - 1], writes=[HB])
                for j in range(NT):
                    c = NT + j
                    cs = slice(c * 128, (c + 1) * 128)
                    r_t, r_b = rhsr.next()
                    S.op("dve", lambda r_t=r_t, c=c: nc.vector.tensor_tensor(
                        out=r_t[:, :, :], in0=tri_f[:, :].unsqueeze(1).to_broadcast([128, 4, 128]),
                        in1=dA_all[:, c, hs4].unsqueeze(2).to_broadcast([128, 4, 128]), op=ALU.mult),
                        reads=[CB, PB], writes=[r_b])
                    bank = j % 2
                    S.op("pe", lambda r_t=r_t, bank=bank: nc.tensor.matmul(
                        ps[bank][:], lhsT=ones_f[:], rhs=r_t[:, :, :].rearrange("p h l -> p (h l)"), start=True, stop=False),
                        reads=[r_b, CB], writes=[pb[bank]], inc=False)
                    S.op("pe", lambda bank=bank: nc.tensor.matmul(
                        ps[bank][:], lhsT=ident_f[:], rhs=negm4[:, :, :].rearrange("p h l -> p (h l)"), start=False, stop=True),
                        reads=[CB, PB], writes=[pb[bank]])
                    sg_t, sg_b = segr.next()
                    for hh in range(4):
                        S.op("act", lambda sg_t=sg_t, bank=bank, hh=hh, c=c, g=g: nc.scalar.activation(
                            out=sg_t[:, hh, :], in_=ps[bank][:, hh * 128:(hh + 1) * 128], func=AF.Exp,
                            bias=ncs_all[:, c, 4 * g + hh:4 * g + hh + 1]), reads=[pb[bank], PB], writes=[sg_b])
                    S.op("pe", lambda cs=cs: nc.tensor.matmul(ps[3][:, 0:128], lhsT=xTa[:, 2, cs], rhs=xTa[:, 3, cs], start=True, stop=True),
                         reads=[XA[2], XA[3]], writes=[pb[3]])
                    cb_t, cb_b = cbr.next()
                    S.op("act", lambda cb_t=cb_t: nc.scalar.copy(out=cb_t[:], in_=ps[3][:, 0:128]), reads=[pb[3]], writes=[cb_b])
                    g_t, g_b = gtr.next()
                    S.op("dve", lambda g_t=g_t, cb_t=cb_t, sg_t=sg_t: nc.vector.tensor_tensor(
                        out=g_t[:, :, :], in0=sg_t[:, :, :], in1=cb_t[:, :].unsqueeze(1).to_broadcast([128, 4, 128]), op=ALU.mult),
                        reads=[cb_b, sg_b], writes=[g_b])
                    for hh in range(4):
                        S.op("pe", lambda g_t=g_t, hh=hh, j=j: nc.tensor.matmul(
                            ps[5][:, hh * 64:(hh + 1) * 64], lhsT=g_t[:, hh, :], rhs=xdt[:, j, hh * 64:(hh + 1) * 64], start=True, stop=True),
                            reads=[g_b, XD], writes=[pb[5]], inc=(hh == 3))
                    S.op("pe", lambda cs=cs, j=j: nc.tensor.matmul(ps[4][:, 0:256], lhsT=xTa[:, 3, cs], rhs=hbf_all[:, j, :], start=True, stop=True),
                         reads=[XA[3], HB], writes=[pb[4]])
                    yd_t, yd_b = ydr.next()
                    S.op("act", lambda yd_t=yd_t: nc.scalar.copy(out=yd_t[:], in_=ps[5][:, 0:256]), reads=[pb[5]], writes=[yd_b])
                    S.op("dve", lambda j=j, c=c: nc.vector.tensor_tensor(
                        out=y_all[:, j, :].rearrange("p (h d) -> p h d", h=4),
                        in0=ps[4][:, 0:256].rearrange("p (h d) -> p h d", h=4),
                        in1=ecs_all[:, c, hs4].unsqueeze(2).to_broadcast([128, 4, 64]), op=ALU.mult),
                        reads=[pb[4], PB], writes=[YB[j]])
                    S.op("dve", lambda j=j, yd_t=yd_t: nc.vector.tensor_tensor(out=y_all[:, j, :], in0=y_all[:, j, :], in1=yd_t[:], op=ALU.add),
                         reads=[yd_b, YB[j]], writes=[YB[j]])
                S.op("dve", lambda: nc.vector.tensor_tensor(out=y_all[:, :, :], in0=y_all[:, :, :], in1=dx_all[:, :, :], op=ALU.add),
                     reads=YB + [DX], writes=YB)
                S.op("dve", lambda: nc.vector.tensor_tensor(out=y_all[:, :, :], in0=y_all[:, :, :], in1=zs_g[:, :, :], op=ALU.mult),
                     reads=YB + [ZG], writes=YB)
                S.op("act", lambda: nc.scalar.activation(out=dx_all[:, :, :], in_=y_all[:, :, :], func=AF.Square),
                     reads=YB, writes=[DX])
                S.op("dve", lambda: nc.vector.tensor_reduce(out=ssg[:, 0:NT], in_=dx_all[:, :, :], axis=AX.X, op=ALU.add),
                     reads=[DX], writes=[SG])
                S.op("act", lambda: nc.scalar.activation(out=ssg[:, NT:2 * NT], in_=ssg[:, 0:NT], func=AF.Sqrt, scale=1.0 / 256,
                                                         bias=eps5[:, 0:1]), reads=[SG, CB], writes=[SG])
                S.op("dve", lambda: nc.vector.reciprocal(out=ssg[:, NT:2 * NT], in_=ssg[:, NT:2 * NT]), reads=[SG], writes=[SG])
                S.op("dve", lambda: nc.vector.tensor_tensor(
                    out=y_all[:, :, :], in0=y_all[:, :, :], in1=ssg[:, NT:2 * NT].unsqueeze(2).to_broadcast([128, NT, 256]), op=ALU.mult),
                    reads=YB + [SG], writes=YB)
                S.op("dve", lambda g=g: nc.vector.tensor_tensor(
                    out=yo_g[:, :, :], in0=y_all[:, :, :],
                    in1=nw_all[:, g * 256:(g + 1) * 256].unsqueeze(1).to_broadcast([128, NT, 256]), op=ALU.mult),
                    reads=YB + [PB], writes=[YO])
                S.dma("sp", lambda g=g: nc.sync.dma_start(
                    out=mix_d[:, 2048 + g * 256:2048 + (g + 1) * 256].rearrange("(t p) d -> p t d", p=128), in_=yo_g[:]),
                    ych, reads=[YO])
            S.barrier()

    if not dbg.get("skip_proj"):
        proj_pass(x_pre, pos_pre, False)
    if stop not in ("A", "Bpre") and not dbg.get("skip_proj"):
        proj_pass(x_own, pos_own, True)
    if stop not in ("A", "Bpre", "B"):
        if not dbg.get("skip_attn"):
            attention()
        if not dbg.get("skip_ssd"):
            ssd2() if not dbg.get("old_ssd") else ssd()

    ssp = sb(es, "ssp", [128, NT, 8], F32)
    SSP = Buf("ssp")
    gate_all = sb(es, "gate_all", [128, NT, 2], F32)
    dest_all = sb(es, "dest_all", [128, NT, 2], I32)
    GD = Buf("gates_dest")

    def wout():
        with ExitStack() as st:
            mixT = sb(st, "mixT", [128, KC, NTOK], BF16)
            mTb = [Buf("mixT%d" % i) for i in range(NT)]
            with ExitStack() as sa:
                mr = Rot(S, sa, "mixrow", [128, D], BF16, 2, dma=True)
                for tt in range(NT):
                    m_t, m_b, m_ch = mr.next()
                    S.dma("sp", lambda m_t=m_t, tt=tt: nc.sync.dma_start(out=m_t[:], in_=mix_d[tt * 128:(tt + 1) * 128, :]),
                          m_ch, writes=[m_b])
                    transpose_rows(m_t, m_b, mixT, mTb[tt], tt)
                S.barrier()
            wv = w_out.rearrange("(kc p) n -> p kc n", p=128)
            wt = Rot(S, st, "wot", [128, KC, 512], BF16, 2, dma=True)
            xr = Rot(S, st, "xres", [128, 512], F32, 3, dma=True)
            x2r = Rot(S, st, "x2t", [128, 512], F32, 3, dma=True)
            junk2 = sb(st, "junk3", [128, 512], BF16)
            J2 = Buf("junk3")
            nbank = 0
            for cb in range(8):
                w_t, w_b, w_ch = wt.next()
                S.dma("pool", lambda w_t=w_t, cb=cb: nc.gpsimd.dma_start(out=w_t[:], in_=wv[:, :, cb * 512:(cb + 1) * 512]),
                      w_ch, writes=[w_b])
                for tt in range(NT):
                    x_t, x_b, x_ch = xr.next()
                    S.dma("sp", lambda x_t=x_t, tt=tt, cb=cb: nc.sync.dma_start(
                        out=x_t[:], in_=x_own[tt * 128:(tt + 1) * 128, cb * 512:(cb + 1) * 512]), x_ch, writes=[x_b])
                    bank = nbank % 4
                    nbank += 1
                    for kc in range(KC):
                        S.op("pe", lambda bank=bank, kc=kc, tt=tt, w_t=w_t: nc.tensor.matmul(
                            ps[bank][:], lhsT=mixT[:, kc, tt * 128:(tt + 1) * 128], rhs=w_t[:, kc, :],
                            start=(kc == 0), stop=(kc == KC - 1)), reads=[w_b, mTb[tt]], writes=[pb[bank]], inc=(kc == KC - 1))
                    o_t, o_b, o_ch = x2r.next()
                    S.op("dve", lambda o_t=o_t, bank=bank, x_t=x_t: nc.vector.tensor_tensor(out=o_t[:], in0=ps[bank][:], in1=x_t[:], op=ALU.add),
                         reads=[pb[bank], x_b], writes=[o_b])
                    S.op("act", lambda o_t=o_t, tt=tt, cb=cb: nc.scalar.activation(out=junk2[:], in_=o_t[:], func=AF.Square,
                                                                                  accum_out=ssp[:, tt, cb:cb + 1]),
                         reads=[o_b], writes=[J2, SSP])
                    S.dma("sp", lambda o_t=o_t, tt=tt, cb=cb: nc.sync.dma_start(
                        out=x2_d[tt * 128:(tt + 1) * 128, cb * 512:(cb + 1) * 512], in_=o_t[:]), o_ch, reads=[o_b])
            S.barrier()

    def route():
        with ExitStack() as st:
            wr = sb(st, "wr", [128, KC, 36], F32)
            nfc = sb(st, "nfc", [128, KC], F32)
            brb = sb(st, "brb", [128, 36], F32)
            ebase = sb(st, "ebase", [128, NE], F32)
            wbc2 = sb(st, "wbc2", [128, D], F32)
            RP = Buf("route_params")
            rpc = S.chan()
            S.dma("sp", lambda: nc.sync.dma_start(out=wr[:], in_=w_r), rpc, writes=[RP])
            S.dma("sp", lambda: nc.sync.dma_start(out=nfc[:], in_=nfw_col), rpc, writes=[RP])
            S.dma("sp", lambda: nc.sync.dma_start(out=brb[:], in_=b_r.partition_broadcast(128)), rpc, writes=[RP])
            S.dma("sp", lambda: nc.sync.dma_start(out=ebase[:], in_=c_ebase), rpc, writes=[RP])
            S.dma("sp", lambda: nc.sync.dma_start(out=wbc2[:], in_=norm_ffn_w.partition_broadcast(128)), rpc, writes=[RP])
            S.op("dve", lambda: nc.vector.tensor_tensor(out=wr[:], in0=wr[:], in1=nfc[:, :].unsqueeze(2).to_broadcast([128, KC, 36]),
                                                        op=ALU.mult), reads=[RP], writes=[RP])
            bg_convert(NCV_CHUNKS)
            x2r = Rot(S, st, "x2row", [128, D], F32, 2, dma=True)
            x2T = sb(st, "x2T", [128, KC, 128], F32)
            XTB = Buf("x2T")
            h2r = Rot(S, st, "h2b", [128, D], BF16, 2, dma=True)
            oh_all = sb(st, "oh_all", [128, NT, NE], BF16)
            OHB = [Buf("oh%d" % i) for i in range(NT)]
            sm = Rot(S, st, "rsm", [128, 160], F32, 2)
            for tt in range(NT):
                x_t, x_b, x_ch = x2r.next()
                S.dma("sp", lambda x_t=x_t, tt=tt: nc.sync.dma_start(out=x_t[:], in_=x2_d[tt * 128:(tt + 1) * 128, :]), x_ch, writes=[x_b])
                t_, tb = sm.next()
                V = lambda a, b: t_[:, a:b]
                dv = lambda fn, rd=(), wr_=None: S.op("dve", fn, reads=[tb] + list(rd), writes=[tb] + list(wr_ or []))
                dv(lambda: nc.vector.tensor_reduce(out=V(0, 1), in_=ssp[:, tt, :], axis=AX.X, op=ALU.add), rd=[SSP])
                S.op("act", lambda: nc.scalar.activation(out=V(1, 2), in_=V(0, 1), func=AF.Sqrt, scale=1.0 / D, bias=eps6[:, 0:1]),
                     reads=[tb, CB], writes=[tb])
                dv(lambda: nc.vector.reciprocal(out=V(1, 2), in_=V(1, 2)))
                h_t, h_b, h_ch = h2r.next()
                S.op("dve", lambda: nc.vector.scalar_tensor_tensor(out=h_t[:], in0=x_t[:], scalar=V(1, 2), in1=wbc2[:],
                                                                   op0=ALU.mult, op1=ALU.mult), reads=[x_b, tb, RP], writes=[h_b])
                for q8 in range(8):
                    bank = q8 % 4
                    for j in range(4):
                        kc = q8 * 4 + j
                        S.op("pe", lambda bank=bank, j=j, kc=kc: nc.tensor.transpose(
                            out=ps[bank][:, j * 128:(j + 1) * 128], in_=x_t[:, kc * 128:(kc + 1) * 128], identity=ident_f[:]),
                            reads=[x_b, CB], writes=[pb[bank]], inc=(j == 3))
                    src = ps[bank][:].rearrange("p (j t) -> p j t", j=4)
                    dst = x2T[:, q8 * 4:(q8 + 1) * 4, :]
                    if q8 % 2 == 0:
                        S.op("act", lambda dst=dst, src=src: nc.scalar.copy(out=dst, in_=src), reads=[pb[bank]], writes=[XTB])
                    else:
                        S.op("dve", lambda dst=dst, src=src: nc.vector.tensor_copy(out=dst, in_=src), reads=[pb[bank]], writes=[XTB])
                for kc in range(KC):
                    S.op("pe", lambda kc=kc: nc.tensor.matmul(ps[4][:, 0:36], lhsT=x2T[:, kc, :], rhs=wr[:, kc, :],
                                                              start=(kc == 0), stop=(kc == KC - 1)),
                         reads=[XTB, RP], writes=[pb[4]], inc=(kc == KC - 1))
                dv(lambda: nc.vector.scalar_tensor_tensor(out=V(4, 40), in0=ps[4][:, 0:36], scalar=V(1, 2), in1=brb[:],
                                                          op0=ALU.mult, op1=ALU.add), rd=[pb[4], RP])
                LG = (4, 8)
                LE = (8, 40)
                dv(lambda: nc.vector.tensor_reduce(out=V(2, 3), in_=V(*LG), axis=AX.X, op=ALU.max))
                dv(lambda: nc.vector.tensor_scalar(out=V(40, 44), in0=V(*LG), scalar1=V(2, 3), scalar2=None, op0=ALU.is_equal))
                dv(lambda: nc.vector.tensor_scalar(out=V(3, 4), in0=V(2, 3), scalar1=-1.0, scalar2=None, op0=ALU.mult))
                S.op("act", lambda: nc.scalar.activation(out=V(44, 48), in_=V(*LG), func=AF.Exp, bias=V(3, 4), accum_out=V(48, 49)),
                     reads=[tb], writes=[tb])
                dv(lambda: nc.vector.reciprocal(out=V(49, 50), in_=V(48, 49)))
                dv(lambda: nc.vector.tensor_scalar(out=V(50, 54), in0=V(40, 44), scalar1=-1.0, scalar2=1e30, op0=ALU.add, op1=ALU.mult))
                dv(lambda: nc.vector.tensor_tensor(out=V(56, 88).rearrange("p (g e) -> p g e", g=4),
                                                   in0=V(*LE).rearrange("p (g e) -> p g e", g=4),
                                                   in1=V(50, 54).unsqueeze(2).to_broadcast([128, 4, 8]), op=ALU.add))
                dv(lambda: nc.vector.tensor_reduce(out=V(54, 55), in_=V(56, 88), axis=AX.X, op=ALU.max))
                dv(lambda: nc.vector.tensor_scalar(out=V(88, 120), in0=V(56, 88), scalar1=V(54, 55), scalar2=None, op0=ALU.is_equal))
                dv(lambda: nc.vector.scalar_tensor_tensor(out=V(56, 88), in0=V(88, 120), scalar=-1e30, in1=V(56, 88),
                                                          op0=ALU.mult, op1=ALU.add))
                dv(lambda: nc.vector.tensor_reduce(out=V(55, 56), in_=V(56, 88), axis=AX.X, op=ALU.max))
                dv(lambda: nc.vector.tensor_scalar(out=V(120, 152), in0=V(56, 88), scalar1=V(55, 56), scalar2=None, op0=ALU.is_equal))
                dv(lambda: nc.vector.tensor_tensor(out=V(152, 153), in0=V(55, 56), in1=V(54, 55), op=ALU.subtract))
                S.op("act", lambda: nc.scalar.activation(out=V(153, 154), in_=V(152, 153), func=AF.Exp), reads=[tb], writes=[tb])
                dv(lambda: nc.vector.tensor_scalar(out=V(154, 155), in0=V(153, 154), scalar1=1.0, scalar2=None, op0=ALU.add))
                dv(lambda: nc.vector.reciprocal(out=V(154, 155), in_=V(154, 155)))
                dv(lambda: nc.vector.tensor_tensor(out=V(155, 156), in0=V(153, 154), in1=V(154, 155), op=ALU.mult))
                dv(lambda: nc.vector.tensor_scalar(out=V(156, 158), in0=V(154, 156), scalar1=V(49, 50), scalar2=None, op0=ALU.mult))
                S.op("dve", lambda: nc.vector.tensor_tensor(out=oh_all[:, tt, :], in0=V(88, 120), in1=V(120, 152), op=ALU.add),
                     reads=[tb], writes=[OHB[tt]])
                for j in range(tt + 1):
                    S.op("pe", lambda j=j: nc.tensor.matmul(ps[5][:, 0:NE], lhsT=(stri_b[:] if j == tt else ones_b[:]),
                                                            rhs=oh_all[:, j, :], start=(j == 0), stop=(j == tt)),
                         reads=[OHB[j], CB], writes=[pb[5]], inc=(j == tt))
                dv(lambda: nc.vector.tensor_copy(out=V(56, 88), in_=ps[5][:, 0:NE]), rd=[pb[5]])
                dv(lambda: nc.vector.tensor_tensor(out=V(4, 36), in0=V(56, 88), in1=ebase[:], op=ALU.add), rd=[RP])
                for k, (o0, o1) in enumerate(((88, 120), (120, 152))):
                    dv(lambda o0=o0, o1=o1: nc.vector.tensor_tensor(out=V(o0, o1), in0=V(o0, o1), in1=V(4, 36), op=ALU.mult))
                    dv(lambda o0=o0, o1=o1: nc.vector.tensor_reduce(out=V(0, 1), in_=V(o0, o1), axis=AX.X, op=ALU.add))
                    dv(lambda o0=o0, o1=o1: nc.vector.tensor_tensor(out=V(o0, o1), in0=V(o0, o1), in1=ebase[:], op=ALU.subtract), rd=[RP])
                    dv(lambda o0=o0, o1=o1: nc.vector.tensor_reduce(out=V(2, 3), in_=V(o0, o1), axis=AX.X, op=ALU.max))
                    dv(lambda: nc.vector.tensor_scalar(out=V(2, 3), in0=V(2, 3), scalar1=float(CE), scalar2=None, op0=ALU.is_ge))
                    dv(lambda: nc.vector.scalar_tensor_tensor(out=V(0, 1), in0=V(2, 3), scalar=1e6, in1=V(0, 1), op0=ALU.mult, op1=ALU.add))
                    dv(lambda: nc.vector.tensor_scalar(out=V(2, 3), in0=V(2, 3), scalar1=-1.0, scalar2=1.0, op0=ALU.mult, op1=ALU.add))
                    dv(lambda k=k: nc.vector.tensor_tensor(out=gate_all[:, tt, k:k + 1], in0=V(156 + k, 157 + k), in1=V(2, 3), op=ALU.mult), wr_=[GD])
                    dv(lambda k=k: nc.vector.tensor_copy(out=dest_all[:, tt, k:k + 1], in_=V(0, 1)), wr_=[GD])
                    S.dma("pool", lambda k=k: nc.gpsimd.indirect_dma_start(
                        out=xs_d[:, :], out_offset=bass.IndirectOffsetOnAxis(ap=dest_all[:, tt, k:k + 1], axis=0),
                        in_=h_t[:, :], in_offset=None, bounds_check=NE * CE - 1, oob_is_err=False), h_ch, reads=[h_b, GD])
            S.barrier()

    def experts():
        elist = dbg.get("experts", list(range(NE)))
        with ExitStack() as st:
            xgr = Rot(S, st, "xg", [128, D], BF16, 2, dma=True)
            xTr = Rot(S, st, "xgT", [128, KC, 128], BF16, 2)
            wgu = Rot(S, st, "wgu", [128, KC, 512], BF16, 2, dma=True)
            wdr = Rot(S, st, "wdn", [128, 8, 1024], BF16, 2, dma=True)
            gsr = Rot(S, st, "gsb", [128, 512], F32, 2)
            acr = Rot(S, st, "actv", [128, FF], BF16, 2)
            aTr = Rot(S, st, "actT", [128, 8, 128], BF16, 2)
            ysr = Rot(S, st, "yst", [128, 512], F32, 3, dma=True)
            for wi, e in enumerate(elist):
                x_t, x_b, x_ch = xgr.next()
                S.dma("sp", lambda x_t=x_t, e=e: nc.sync.dma_start(out=x_t[:], in_=xs_d[e * CE:(e + 1) * CE, :]), x_ch, writes=[x_b])
                xT_t, xT_b = xTr.next()
                transpose_rows(x_t, x_b, xT_t, xT_b, 0, evac_alt=wi)
                a_t, a_b = acr.next()
                wgv = w_gate[wi].rearrange("(kc p) f -> p kc f", p=128)
                wuv = w_up[wi].rearrange("(kc p) f -> p kc f", p=128)
                for half in range(2):
                    hs = slice(half * 512, (half + 1) * 512)
                    g_w, g_wb, g_ch = wgu.next()
                    cbf = conv_bufs.get((wi, "g", half))
                    if cbf is not None:
                        gsrc = wbg_d[wi].rearrange("(kc p) f -> p kc f", p=128)[:, :, hs]
                        S.dma("pool", lambda g_w=g_w, gsrc=gsrc: nc.gpsimd.dma_start(out=g_w[:], in_=gsrc), g_ch, reads=[cbf], writes=[g_wb])
                    else:
                        S.dma("pool", lambda g_w=g_w, hs=hs, wgv=wgv: nc.gpsimd.dma_start(out=g_w[:], in_=wgv[:, :, hs]), g_ch, writes=[g_wb])
                    for kc in range(KC):
                        S.op("pe", lambda kc=kc, g_w=g_w: nc.tensor.matmul(ps[4][:], lhsT=xT_t[:, kc, :], rhs=g_w[:, kc, :],
                                                                          start=(kc == 0), stop=(kc == KC - 1)),
                             reads=[xT_b, g_wb], writes=[pb[4]], inc=(kc == KC - 1))
                    u_w, u_wb, u_ch = wgu.next()
                    cbf = conv_bufs.get((wi, "u", half))
                    if cbf is not None:
                        usrc = wbu_d[wi].rearrange("(kc p) f -> p kc f", p=128)[:, :, hs]
                        S.dma("pool", lambda u_w=u_w, usrc=usrc: nc.gpsimd.dma_start(out=u_w[:], in_=usrc), u_ch, reads=[cbf], writes=[u_wb])
                    else:
                        S.dma("pool", lambda u_w=u_w, hs=hs, wuv=wuv: nc.gpsimd.dma_start(out=u_w[:], in_=wuv[:, :, hs]), u_ch, writes=[u_wb])
                    for kc in range(KC):
                        S.op("pe", lambda kc=kc, u_w=u_w: nc.tensor.matmul(ps[5][:], lhsT=xT_t[:, kc, :], rhs=u_w[:, kc, :],
                                                                          start=(kc == 0), stop=(kc == KC - 1)),
                             reads=[xT_b, u_wb], writes=[pb[5]], inc=(kc == KC - 1))
                    gs_t, gs_b = gsr.next()
                    S.op("act", lambda gs_t=gs_t: nc.scalar.activation(out=gs_t[:], in_=ps[4][:], func=AF.Silu), reads=[pb[4]], writes=[gs_b])
                    S.op("dve", lambda gs_t=gs_t, hs=hs: nc.vector.tensor_tensor(out=a_t[:, hs], in0=gs_t[:], in1=ps[5][:], op=ALU.mult),
                         reads=[gs_b, pb[5]], writes=[a_b])
                aT_t, aT_b = aTr.next()
                pbf = ps[6][:].bitcast(BF16)
                for j in range(8):
                    S.op("pe", lambda j=j: nc.tensor.transpose(out=pbf[:, j * 128:(j + 1) * 128], in_=a_t[:, j * 128:(j + 1) * 128],
                                                               identity=ident_b[:]), reads=[a_b, CB], writes=[pb[6]], inc=(j == 7))
                S.op("act", lambda: nc.scalar.copy(out=aT_t[:], in_=pbf.rearrange("p (j t) -> p j t", j=8)), reads=[pb[6]], writes=[aT_b])
                wdv = w_down[wi].rearrange("(fk p) d -> p fk d", p=128)
                for q in range(4):
                    d_w, d_wb, d_ch = wdr.next()
                    cbf = conv_bufs.get((wi, "d", q // 2))
                    if cbf is not None:
                        dsrc = wbd_d[wi].rearrange("(fk p) d -> p fk d", p=128)[:, :, q * 1024:(q + 1) * 1024]
                        S.dma("pool", lambda d_w=d_w, dsrc=dsrc: nc.gpsimd.dma_start(out=d_w[:], in_=dsrc), d_ch, reads=[cbf], writes=[d_wb])
                    else:
                        S.dma("pool", lambda d_w=d_w, q=q, wdv=wdv: nc.gpsimd.dma_start(out=d_w[:], in_=wdv[:, :, q * 1024:(q + 1) * 1024]),
                              d_ch, writes=[d_wb])
                    for cbk in range(2):
                        bank = (q * 2 + cbk) % 4
                        for fk in range(8):
                            S.op("pe", lambda fk=fk, bank=bank, cbk=cbk, d_w=d_w: nc.tensor.matmul(
                                ps[bank][:], lhsT=aT_t[:, fk, :], rhs=d_w[:, fk, cbk * 512:(cbk + 1) * 512],
                                start=(fk == 0), stop=(fk == 7)), reads=[aT_b, d_wb], writes=[pb[bank]], inc=(fk == 7))
                        y_t, y_b, y_ch = ysr.next()
                        if cbk == 0:
                            S.op("act", lambda y_t=y_t, bank=bank: nc.scalar.copy(out=y_t[:], in_=ps[bank][:]), reads=[pb[bank]], writes=[y_b])
                        else:
                            S.op("dve", lambda y_t=y_t, bank=bank: nc.vector.tensor_copy(out=y_t[:], in_=ps[bank][:]), reads=[pb[bank]], writes=[y_b])
                        c0 = q * 1024 + cbk * 512
                        S.dma("sp", lambda y_t=y_t, e=e, c0=c0: nc.sync.dma_start(out=ys_d[e * CE:(e + 1) * CE, c0:c0 + 512], in_=y_t[:]),
                              y_ch, reads=[y_b])
            S.barrier()

    def combine():
        with ExitStack() as st:
            y1r = Rot(S, st, "y1", [128, D], F32, 3, dma=True)
            y2r = Rot(S, st, "y2", [128, D], F32, 3, dma=True)
            xr = Rot(S, st, "x2o", [128, D], F32, 3, dma=True)
            for r in (y1r, y2r):
                for i in range(3):
                    S.op("dve", lambda r=r, i=i: nc.vector.memset(r.t[i][:], 0.0), writes=[r.b[i]])
            for tt in range(NT):
                x_t, x_b, x_ch = xr.next()
                S.dma("sp", lambda x_t=x_t, tt=tt: nc.sync.dma_start(out=x_t[:], in_=x2_d[tt * 128:(tt + 1) * 128, :]), x_ch, writes=[x_b])
                ys = []
                for k, rr in enumerate((y1r, y2r)):
                    y_t, y_b, y_ch = rr.next()
                    S.dma("pool", lambda y_t=y_t, k=k, tt=tt: nc.gpsimd.indirect_dma_start(
                        out=y_t[:, :], out_offset=None, in_=ys_d[:, :],
                        in_offset=bass.IndirectOffsetOnAxis(ap=dest_all[:, tt, k:k + 1], axis=0),
                        bounds_check=NE * CE - 1, oob_is_err=False), y_ch, reads=[GD], writes=[y_b])
                    ys.append((y_t, y_b))
                for k, (y_t, y_b) in enumerate(ys):
                    S.op("dve", lambda y_t=y_t, k=k, x_t=x_t, tt=tt: nc.vector.scalar_tensor_tensor(
                        out=x_t[:], in0=y_t[:], scalar=gate_all[:, tt, k:k + 1], in1=x_t[:], op0=ALU.mult, op1=ALU.add),
                        reads=[y_b, GD, x_b], writes=[x_b])
                S.dma("sp", lambda x_t=x_t, tt=tt: nc.sync.dma_start(out=out_d[tt * 128:(tt + 1) * 128, :], in_=x_t[:]), x_ch, reads=[x_b])
            S.barrier()

    if stop not in ("A", "Bpre", "B", "D"):
        if not dbg.get("skip_wout"):
            wout()
        route()
        if stop != "E":
            experts()
            combine()

    S.barrier(final=True)
    es.close()
    return nc, S


def _consts():
    p = np.arange(128)
    c = {}
    c["c_ident"] = np.eye(128, dtype=np.float32)
    sw = np.zeros((128, 128), np.float32)
    sw[(p + 64) % 128, p] = 1.0
    c["c_pswap"] = sw
    c["c_tri"] = (p[:, None] <= p[None, :]).astype(np.float32)
    c["c_stri"] = (p[:, None] < p[None, :]).astype(np.float32)
    c["c_negm"] = np.where(p[None, :] < p[:, None], -30000.0, 0.0).astype(np.float32)
    kj = p[:, None]
    am = np.zeros((128, 16, 128), np.float32)
    for Dd in range(16):
        d = Dd * 128 + p[None, :] - kj
        m = ((d >= 0) & (d <= 128)).astype(np.float32)
        m += ((d >= 0) & (d % 4 == 0) & (d <= 512))
        m += ((d >= 0) & (d % 16 == 0) & (d <= 2048))
        am[:, Dd, :] = m
    c["c_amask"] = am.reshape(128, 2048)
    inv = np.power(np.float32(10000.0), -np.arange(64, dtype=np.float32) / 64).astype(np.float32)
    rope = np.zeros((128, 2), np.float32)
    rope[:, 0] = np.tile(inv, 2) / np.float32(2 * np.pi)
    rope[:64, 1] = -TWO_PI
    rope[64:, 1] = TWO_PI
    c["c_rope"] = rope
    c["c_ebase"] = np.tile((np.arange(NE) * CE).astype(np.float32)[None, :], (128, 1))
    return c


def make_in_maps(inp):
    f = lambda a: np.ascontiguousarray(np.asarray(a, dtype=np.float32))
    x = f(inp["x"])
    pos = np.ascontiguousarray(np.asarray(inp["positions"], dtype=np.int32))
    shared = {
        "w_in": f(inp["w_in"][0]), "w_out": f(inp["w_out"][0]),
        "w_gate": f(inp["w_gate"][0]), "w_up": f(inp["w_up"][0]), "w_down": f(inp["w_down"][0]),
        "norm_attn_w": f(inp["norm_attn_w"][0]), "norm_ffn_w": f(inp["norm_ffn_w"][0]),
        "nfw_col": f(np.asarray(inp["norm_ffn_w"][0]).reshape(KC, 128).T),
        "qk_w": f(np.stack([np.asarray(inp["q_norm_w"][0]), np.asarray(inp["k_norm_w"][0])], axis=1)),
        "conv_wc": f(np.asarray(inp["conv_w"][0]).reshape(4, 32, 128).transpose(2, 1, 0)),
        "conv_bc": f(np.asarray(inp["conv_b"][0]).reshape(32, 128).T),
        "dt_bias": f(inp["dt_bias"][0]), "a_log": f(inp["a_log"][0]),
        "d_rep": f(np.repeat(np.asarray(inp["d_skip"][0]), 64)),
        "ssd_nw": f(inp["ssd_norm_w"][0]),
        "w_r": f(np.concatenate([np.asarray(inp["router_group_w"][0]), np.asarray(inp["router_expert_w"][0])],
                                axis=1).reshape(KC, 128, 36).transpose(1, 0, 2)),
        "b_r": f(np.concatenate([np.asarray(inp["router_group_b"][0]), np.asarray(inp["router_expert_b"][0])])),
    }
    shared.update(_consts())
    maps = []
    for c in range(8):
        b, th = c // 2, c % 2
        m = dict(shared)
        m["x_own"] = np.ascontiguousarray(x[b, th * NTOK:(th + 1) * NTOK])
        m["pos_own"] = np.ascontiguousarray(pos[b, th * NTOK:(th + 1) * NTOK])
        if th == 1:
            m["x_pre"] = np.ascontiguousarray(x[b, 0:NTOK])
            m["pos_pre"] = np.ascontiguousarray(pos[b, 0:NTOK])
            m["vflag"] = np.ones((128, NT), np.float32)
        else:
            m["x_pre"] = np.zeros((NTOK, D), np.float32)
            m["pos_pre"] = np.zeros((NTOK,), np.int32)
            m["vflag"] = np.zeros((128, NT), np.float32)
        maps.append(m)
    return maps


_PROG = {}


def kernel(**inputs):
    if "nc" not in _PROG:
        _PROG["nc"] = build_program()[0]
    maps = make_in_maps(inputs)
    res = run_bass_kernel_spmd(_PROG["nc"], maps, core_ids=list(range(8)))
    out = np.zeros((4, 2 * NTOK, D), np.float32)
    for c in range(8):
        b, th = c // 2, c % 2
        out[b, th * NTOK:(th + 1) * NTOK] = res.results[c]["out"]
    return out
```
